# Optimizing a Trainium2 kernel written in Bass

```python
import jax
import jax.numpy as jnp
from jax import lax
import numpy as np

D_MODEL = 1024
BATCH = 8
SEQ = 4096
DEPTH = 2

GRID_W = 64
CTX_LEN = 256
EPS = 1e-6

MLA_HEADS = 6
Q_LORA = 384
KV_LORA = 256
NOPE_DIM = 128
ROPE_DIM = 64
V_DIM = 128
QK_DIM = NOPE_DIM + ROPE_DIM
ROPE_BASE = 10000.0
Q_BLOCK = 128
FOURIER_GROUPS = 4
FOURIER_GROUP_DIM = 64
FOURIER_WIDTH = FOURIER_GROUPS * FOURIER_GROUP_DIM
MLA_IN = Q_LORA + KV_LORA + ROPE_DIM
EVEN_IN = MLA_IN + FOURIER_WIDTH
EVEN_MIX = MLA_HEADS * V_DIM + FOURIER_WIDTH
GLA_HEADS = 4
GLA_DK = 128
GLA_DV = 256
GATE_RANK = 16
GATE_NORMALIZER = 16.0
GLA_CHUNK = 64
GLA_KW = GLA_HEADS * GLA_DK
GLA_VW = GLA_HEADS * GLA_DV
ODD_IN = 2 * GLA_KW + 2 * GLA_VW + 2 * GATE_RANK
ODD_MIX = GLA_VW
N_EXPERTS = 32
TOP_K = 4
D_FF = 1024
SWIGLU_ALPHA = 1.702
SWIGLU_LIMIT = 7.0
EXPERT_BLOCK = 128

kernel_name = "hybrid_mla_fnet_gla_moe_dit"


def rmsnorm(x, g):
    xf = x.astype(jnp.float32)
    y = xf * lax.rsqrt(jnp.mean(xf * xf, axis=-1, keepdims=True) + EPS)
    return (y * g.astype(jnp.float32)).astype(x.dtype)


def modulate(x, g, shift, scale):
    return rmsnorm(x, g) * (1 + scale) + shift


def adaln(cvec, mod_w, mod_b):
    m = jax.nn.silu(cvec) @ mod_w + mod_b
    return jnp.split(m[..., None, :], 6, axis=-1)


def _flip(a):
    return a[:, ::-1]


def axial_rope_tables(n_tokens):
    rows = n_tokens // GRID_W
    row, col = jnp.meshgrid(jnp.arange(rows, dtype=jnp.float32),
                            jnp.arange(GRID_W, dtype=jnp.float32), indexing="ij")
    axis_dim = ROPE_DIM // 2
    inv_freq = ROPE_BASE ** (-jnp.arange(0, axis_dim, 2, dtype=jnp.float32) / axis_dim)
    ang_r = row.reshape(-1, 1) * inv_freq
    ang_c = col.reshape(-1, 1) * inv_freq
    ang = jnp.concatenate([ang_r, ang_r, ang_c, ang_c], axis=-1)
    return jnp.cos(ang), jnp.sin(ang)


def apply_rope(x, cos, sin):
    r1, r2, c1, c2 = jnp.split(x, 4, axis=-1)
    rot = jnp.concatenate([-r2, r1, -c2, c1], axis=-1)
    return (x * cos + rot * sin).astype(x.dtype)


def mla_queries(c_q, q_norm_g, w_q_up, cos, sin):
    b, n, _ = c_q.shape
    q = (rmsnorm(c_q, q_norm_g) @ w_q_up).reshape(b, n, MLA_HEADS, QK_DIM)
    q_nope, q_rope = q[..., :NOPE_DIM], q[..., NOPE_DIM:]
    if cos is not None:
        q_rope = apply_rope(q_rope, cos[:, None, :], sin[:, None, :])
    return jnp.concatenate([q_nope, q_rope], axis=-1)


def mla_keys_values(u_kv, kv_norm_g, w_kv_up, cos, sin):
    b, n, _ = u_kv.shape
    c_kv, k_rope = u_kv[..., :KV_LORA], u_kv[..., KV_LORA:]
    kv = (rmsnorm(c_kv, kv_norm_g) @ w_kv_up).reshape(b, n, MLA_HEADS, NOPE_DIM + V_DIM)
    k_nope, v = kv[..., :NOPE_DIM], kv[..., NOPE_DIM:]
    if cos is not None:
        k_rope = apply_rope(k_rope, cos, sin)
    k = jnp.concatenate([k_nope, jnp.broadcast_to(k_rope[:, :, None, :], (b, n, MLA_HEADS, ROPE_DIM))], axis=-1)
    return k, v


def attend(q, k, v):
    s = jnp.einsum("bqhd,bkhd->bhqk", q.astype(jnp.float32), k.astype(jnp.float32)) * (QK_DIM ** -0.5)
    p = jax.nn.softmax(s, axis=-1).astype(v.dtype)
    return jnp.einsum("bhqk,bkhd->bqhd", p, v)


def blocked_attend(q, k, v):
    b, n, h, dk = q.shape
    nb = n // Q_BLOCK
    qb = q.reshape(b, nb, Q_BLOCK, h, dk).transpose(1, 0, 2, 3, 4)
    ob = lax.map(lambda qi: attend(qi, k, v), qb)
    return ob.transpose(1, 0, 2, 3, 4).reshape(b, n, h, -1)


def fourier_mix(u):
    b, n, _ = u.shape
    ug = u.reshape(b, n, FOURIER_GROUPS, FOURIER_GROUP_DIM).astype(jnp.float32)
    f = jnp.fft.fft2(ug, axes=(1, 3), norm="ortho").real
    return f.reshape(b, n, FOURIER_WIDTH).astype(u.dtype)


def even_mixer(h_lat, h_ctx, w_in, q_norm_g, w_q_up, kv_norm_g, w_kv_up, w_out, ctx_out):
    b, n, _ = h_lat.shape
    cos, sin = axial_rope_tables(n)
    u_lat = h_lat @ w_in
    u_ctx = h_ctx @ w_in
    q_lat = mla_queries(u_lat[..., :Q_LORA], q_norm_g, w_q_up, cos, sin)
    k_lat, v_lat = mla_keys_values(u_lat[..., Q_LORA:MLA_IN], kv_norm_g, w_kv_up, cos, sin)
    k_ctx, v_ctx = mla_keys_values(u_ctx[..., Q_LORA:MLA_IN], kv_norm_g, w_kv_up, None, None)
    a_lat = blocked_attend(q_lat, jnp.concatenate([k_ctx, k_lat], axis=1),
                           jnp.concatenate([v_ctx, v_lat], axis=1))
    y_lat = jnp.concatenate([a_lat.reshape(b, n, -1), fourier_mix(u_lat[..., MLA_IN:])], axis=-1) @ w_out
    y_ctx = None
    if ctx_out:
        q_ctx = mla_queries(u_ctx[..., :Q_LORA], q_norm_g, w_q_up, None, None)
        a_ctx = attend(q_ctx, k_ctx, v_ctx)
        y_ctx = jnp.concatenate([a_ctx.reshape(b, h_ctx.shape[1], -1), fourier_mix(u_ctx[..., MLA_IN:])], axis=-1) @ w_out
    return y_lat, y_ctx


def gla_project(u, w_gk_fwd, b_gk_fwd, w_gk_bwd, b_gk_bwd):
    b, n, _ = u.shape
    q, k, v, g_out, gd_fwd, gd_bwd = jnp.split(
        u, [GLA_KW, 2 * GLA_KW, 2 * GLA_KW + GLA_VW, 2 * GLA_KW + 2 * GLA_VW,
            2 * GLA_KW + 2 * GLA_VW + GATE_RANK], axis=-1)

    def heads(a, d):
        return a.reshape(b, n, GLA_HEADS, d).astype(jnp.float32)

    def decay(gd, w_up, b_up):
        z = (gd @ w_up + b_up).astype(jnp.float32)
        return heads(jax.nn.log_sigmoid(z) / GATE_NORMALIZER, GLA_DK)

    return (heads(q, GLA_DK) * (GLA_DK ** -0.5), heads(k, GLA_DK), heads(v, GLA_DV), g_out,
            decay(gd_fwd, w_gk_fwd, b_gk_fwd), decay(gd_bwd, w_gk_bwd, b_gk_bwd))


def gla_chunked(q, k, v, g, s0):
    b, n, h, _ = q.shape
    nc = n // GLA_CHUNK

    def to_chunks(a):
        return a.reshape(b, nc, GLA_CHUNK, h, a.shape[-1]).transpose(1, 0, 3, 2, 4)

    tril = jnp.tril(jnp.ones((GLA_CHUNK, GLA_CHUNK), dtype=bool))

    def step(state, inp):
        qc, kc, vc, gc = inp
        cum = jnp.cumsum(gc, axis=2)
        last = cum[:, :, -1:, :]
        q_dec = qc * jnp.exp(cum)
        k_dec = kc * jnp.exp(-cum)
        scores = jnp.where(tril, jnp.einsum("bhid,bhjd->bhij", q_dec, k_dec), 0.0)
        out = jnp.einsum("bhid,bhde->bhie", q_dec, state) + jnp.einsum("bhij,bhje->bhie", scores, vc)
        state = (jnp.exp(last)[:, :, 0, :, None] * state
                 + jnp.einsum("bhjd,bhje->bhde", kc * jnp.exp(last - cum), vc))
        return state, out

    s_fin, o = lax.scan(step, s0, (to_chunks(q), to_chunks(k), to_chunks(v), to_chunks(g)))
    return o.transpose(1, 0, 3, 2, 4).reshape(b, n, h, -1), s_fin


def gla_final_state(k, v, g):
    cum = jnp.cumsum(g, axis=1)
    return jnp.einsum("bnhd,bnhe->bhde", k * jnp.exp(cum[:, -1:] - cum), v)


def gla_output(o, g_out, gnorm_g, w_out):
    b, n = o.shape[:2]
    o = rmsnorm(o, gnorm_g).reshape(b, n, GLA_VW) * jax.nn.silu(g_out.astype(jnp.float32))
    return o.astype(w_out.dtype) @ w_out


def odd_mixer(h_lat, h_ctx, w_in, w_gk_fwd, b_gk_fwd, w_gk_bwd, b_gk_bwd, gnorm_g, w_out, ctx_out):
    gates = (w_gk_fwd, b_gk_fwd, w_gk_bwd, b_gk_bwd)
    q_c, k_c, v_c, go_c, gf_c, gb_c = gla_project(h_ctx @ w_in, *gates)
    y_ctx = None
    if ctx_out:
        zero = jnp.zeros((h_ctx.shape[0], GLA_HEADS, GLA_DK, GLA_DV), jnp.float32)
        o_f, s_fwd = gla_chunked(q_c, k_c, v_c, gf_c, zero)
        o_b, s_bwd = gla_chunked(_flip(q_c), _flip(k_c), _flip(v_c), _flip(gb_c), zero)
        y_ctx = gla_output(o_f + _flip(o_b), go_c, gnorm_g, w_out)
    else:
        s_fwd = gla_final_state(k_c, v_c, gf_c)
        s_bwd = gla_final_state(_flip(k_c), _flip(v_c), _flip(gb_c))
    q, k, v, go, gf, gb = gla_project(h_lat @ w_in, *gates)
    o_f, _ = gla_chunked(q, k, v, gf, s_fwd)
    o_b, _ = gla_chunked(_flip(q), _flip(k), _flip(v), _flip(gb), s_bwd)
    return gla_output(o_f + _flip(o_b), go, gnorm_g, w_out), y_ctx


def clamped_swiglu(gu):
    x_glu, x_lin = gu[..., ::2], gu[..., 1::2]
    x_glu = jnp.minimum(x_glu, SWIGLU_LIMIT)
    x_lin = jnp.clip(x_lin, -SWIGLU_LIMIT, SWIGLU_LIMIT)
    return x_glu * jax.nn.sigmoid(SWIGLU_ALPHA * x_glu) * (x_lin + 1)


def moe_ffn(h, router_w, router_b, w_gu, b_gu, w_down, b_down):
    n_tok, d = h.shape
    logits = (h @ router_w + router_b).astype(jnp.float32)
    top_val, top_idx = lax.top_k(logits, TOP_K)
    gates = jax.nn.softmax(top_val, axis=-1)
    n_rows = n_tok * TOP_K
    e_flat = top_idx.reshape(-1)
    tok_flat = jnp.repeat(jnp.arange(n_tok, dtype=jnp.int32), TOP_K)
    order = jnp.argsort(e_flat)
    e_sorted = e_flat[order]
    counts = jnp.bincount(e_flat, length=N_EXPERTS)
    starts = jnp.cumsum(counts) - counts
    padded = (counts + EXPERT_BLOCK - 1) // EXPERT_BLOCK * EXPERT_BLOCK
    pad_ends = jnp.cumsum(padded)
    pad_starts = pad_ends - padded
    dest = pad_starts[e_sorted] + jnp.arange(n_rows, dtype=jnp.int32) - starts[e_sorted]
    n_blocks = -(-(n_rows + N_EXPERTS * (EXPERT_BLOCK - 1)) // EXPERT_BLOCK)
    n_pad = n_blocks * EXPERT_BLOCK
    row_tok = jnp.full((n_pad,), n_tok, jnp.int32).at[dest].set(tok_flat[order])
    row_gate = jnp.zeros((n_pad,), jnp.float32).at[dest].set(gates.reshape(-1)[order])
    block_e = jnp.minimum(
        jnp.searchsorted(pad_ends, jnp.arange(n_blocks, dtype=jnp.int32) * EXPERT_BLOCK, side="right"),
        N_EXPERTS - 1)
    h_pad = jnp.concatenate([h, jnp.zeros((1, d), h.dtype)], axis=0)
    xb = h_pad[row_tok].reshape(n_blocks, EXPERT_BLOCK, d)

    def expert_block(args):
        xe, e = args
        gu = xe @ w_gu[e] + b_gu[e]
        return clamped_swiglu(gu) @ w_down[e] + b_down[e]

    yb = lax.map(expert_block, (xb, block_e)).reshape(n_pad, d)
    y = jax.ops.segment_sum(yb * row_gate[:, None].astype(yb.dtype), row_tok, num_segments=n_tok + 1)
    return y[:n_tok]


def setup_inputs(seed: int = 0) -> dict:
    key = jax.random.key(seed)
    keys = list(jax.random.split(key, 48))

    def normal(shape, scale=1.0):
        return scale * jax.random.normal(keys.pop(), shape, jnp.float32)

    def dense(shape, fan_in, scale=1.0):
        return normal(shape, scale * fan_in ** -0.5)

    def gain(n):
        return 1.0 + normal((n,), 0.05)

    D, F, E = D_MODEL, D_FF, N_EXPERTS
    inp = {}
    inp["x"] = normal((BATCH, SEQ, D))
    inp["c"] = normal((BATCH, D))
    inp["ctx"] = normal((BATCH, CTX_LEN, D))
    inp["c_ctx"] = normal((D,))
    inp["final_norm_g"] = gain(D)
    inp["l0_mod_w"] = dense((D, 6 * D), D, 0.5)
    inp["l0_mod_b"] = normal((6 * D,), 0.1)
    inp["l0_norm1_g"] = gain(D)
    inp["l0_w_in"] = dense((D, EVEN_IN), D)
    inp["l0_q_norm_g"] = gain(Q_LORA)
    inp["l0_w_q_up"] = dense((Q_LORA, MLA_HEADS * QK_DIM), Q_LORA)
    inp["l0_kv_norm_g"] = gain(KV_LORA)
    inp["l0_w_kv_up"] = dense((KV_LORA, MLA_HEADS * (NOPE_DIM + V_DIM)), KV_LORA)
    inp["l0_w_out"] = dense((EVEN_MIX, D), EVEN_MIX)
    inp["l0_norm2_g"] = gain(D)
    inp["l0_router_w"] = dense((D, E), D)
    inp["l0_router_b"] = normal((E,), 0.01)
    inp["l0_w_gu"] = dense((E, D, 2 * F), D)
    inp["l0_b_gu"] = normal((E, 2 * F), 0.02)
    inp["l0_w_down"] = dense((E, F, D), F)
    inp["l0_b_down"] = normal((E, D), 0.02)
    inp["l1_mod_w"] = dense((D, 6 * D), D, 0.5)
    inp["l1_mod_b"] = normal((6 * D,), 0.1)
    inp["l1_norm1_g"] = gain(D)
    inp["l1_w_in"] = dense((D, ODD_IN), D)
    inp["l1_w_gk_fwd"] = dense((GATE_RANK, GLA_KW), GATE_RANK)
    inp["l1_b_gk_fwd"] = normal((GLA_KW,), 0.1)
    inp["l1_w_gk_bwd"] = dense((GATE_RANK, GLA_KW), GATE_RANK)
    inp["l1_b_gk_bwd"] = normal((GLA_KW,), 0.1)
    inp["l1_gnorm_g"] = gain(GLA_DV)
    inp["l1_w_out"] = dense((ODD_MIX, D), ODD_MIX)
    inp["l1_norm2_g"] = gain(D)
    inp["l1_router_w"] = dense((D, E), D)
    inp["l1_router_b"] = normal((E,), 0.01)
    inp["l1_w_gu"] = dense((E, D, 2 * F), D)
    inp["l1_b_gu"] = normal((E, 2 * F), 0.02)
    inp["l1_w_down"] = dense((E, F, D), F)
    inp["l1_b_down"] = normal((E, D), 0.02)
    return inp


def reference(x, c, ctx, c_ctx, final_norm_g,
              l0_mod_w, l0_mod_b, l0_norm1_g, l0_w_in, l0_q_norm_g, l0_w_q_up, l0_kv_norm_g, l0_w_kv_up,
              l0_w_out, l0_norm2_g, l0_router_w, l0_router_b, l0_w_gu, l0_b_gu, l0_w_down, l0_b_down,
              l1_mod_w, l1_mod_b, l1_norm1_g, l1_w_in, l1_w_gk_fwd, l1_b_gk_fwd, l1_w_gk_bwd, l1_b_gk_bwd,
              l1_gnorm_g, l1_w_out, l1_norm2_g, l1_router_w, l1_router_b, l1_w_gu, l1_b_gu, l1_w_down,
              l1_b_down):
    common = [(l0_mod_w, l0_mod_b, l0_norm1_g, l0_norm2_g),
              (l1_mod_w, l1_mod_b, l1_norm1_g, l1_norm2_g)]
    mixers = [(even_mixer, (l0_w_in, l0_q_norm_g, l0_w_q_up, l0_kv_norm_g, l0_w_kv_up, l0_w_out)),
              (odd_mixer, (l1_w_in, l1_w_gk_fwd, l1_b_gk_fwd, l1_w_gk_bwd, l1_b_gk_bwd, l1_gnorm_g, l1_w_out))]
    experts = [(l0_router_w, l0_router_b, l0_w_gu, l0_b_gu, l0_w_down, l0_b_down),
               (l1_router_w, l1_router_b, l1_w_gu, l1_b_gu, l1_w_down, l1_b_down)]
    x_lat, x_ctx = x, ctx
    for i in range(DEPTH):
        last = i == DEPTH - 1
        mod_w, mod_b, norm1_g, norm2_g = common[i]
        mixer, mixer_params = mixers[i]
        sh1, sc1, g1, sh2, sc2, g2 = adaln(c, mod_w, mod_b)
        csh1, csc1, cg1, csh2, csc2, cg2 = adaln(c_ctx, mod_w, mod_b)
        y_lat, y_ctx = mixer(modulate(x_lat, norm1_g, sh1, sc1), modulate(x_ctx, norm1_g, csh1, csc1),
                             *mixer_params, ctx_out=not last)
        x_lat = x_lat + g1 * y_lat
        h_lat = modulate(x_lat, norm2_g, sh2, sc2)
        b, n, d = h_lat.shape
        if last:
            x_lat = x_lat + g2 * moe_ffn(h_lat.reshape(b * n, d), *experts[i]).reshape(b, n, d)
        else:
            x_ctx = x_ctx + cg1 * y_ctx
            h_ctx = modulate(x_ctx, norm2_g, csh2, csc2)
            m = h_ctx.shape[1]
            y = moe_ffn(jnp.concatenate([h_ctx.reshape(b * m, d), h_lat.reshape(b * n, d)], axis=0), *experts[i])
            x_ctx = x_ctx + cg2 * y[: b * m].reshape(b, m, d)
            x_lat = x_lat + g2 * y[b * m:].reshape(b, n, d)
    return rmsnorm(x_lat, final_norm_g)
```

```python
import numpy as np
import ml_dtypes
from contextlib import ExitStack
import concourse.bass as bass
import concourse.mybir as mybir
from concourse.bass_utils import run_bass_kernel_spmd

F32 = mybir.dt.float32
BF16 = mybir.dt.bfloat16
AF = mybir.ActivationFunctionType
ALU = mybir.AluOpType
AX = mybir.AxisListType

T = 4352
NT = 34
TC = 256
TL = 4096
D = 1024
BLOCKS = [(0, 256)] + [(256 + 512 * i, 512) for i in range(8)]
EPS = 1e-6
NE = 32
ATT_SCALE = 192.0 ** -0.5

STOP_AFTER = None
SUB = 99
L1_ONLY = False
DEBUG_OUT = False

ENGS = ("pe", "act", "dve", "pool", "sp")
N_DMA_SEMS_HW = 48
N_DMA_SEMS_SW = 32
N_DMA_SEMS = N_DMA_SEMS_HW + N_DMA_SEMS_SW


class Prog:
    def __init__(self, nc, es):
        self.nc = nc
        self.streams = {e: [] for e in ENGS}
        self.count = {e: 0 for e in ENGS}
        self.esem = {e: es.enter_context(nc.semaphore("s_" + e)) for e in ENGS if e != "sp"}
        self.dsem = [es.enter_context(nc.semaphore("d%d" % i)) for i in range(N_DMA_SEMS)]
        self.dval = [0] * N_DMA_SEMS
        self.dnext = 0
        self.dnext_sw = 0
        self.waited = {e: {} for e in ENGS}
        self.n_inst = 0

    def _wait(self, eng, tok):
        if tok is None:
            return
        key = (tok[0], tok[1])
        n = tok[2]
        if self.waited[eng].get(key, 0) >= n:
            return
        self.waited[eng][key] = n
        self.streams[eng].append(("wait", key, n))

    def op(self, eng, fn, deps=(), sig=True):
        for d in deps:
            self._wait(eng, d)
        self.n_inst += 1
        if sig:
            self.count[eng] += 1
            self.streams[eng].append(("op", fn, True))
            return ("e", eng, self.count[eng])
        self.streams[eng].append(("op", fn, False))
        return None

    def dma(self, eng, out, in_, deps=(), **kw):
        for d in deps:
            self._wait(eng, d)
        if eng == "pool":
            idx = N_DMA_SEMS_HW + self.dnext_sw
            self.dnext_sw = (self.dnext_sw + 1) % N_DMA_SEMS_SW
        else:
            idx = self.dnext
            self.dnext = (self.dnext + 1) % N_DMA_SEMS_HW
        if self.dval[idx] > 0:
            self._wait(eng, ("d", idx, self.dval[idx]))
        self.dval[idx] += 16
        self.n_inst += 1
        self.streams[eng].append(("dma", out, in_, idx, kw))
        return ("d", idx, self.dval[idx])

    def barrier(self):
        toks = [("e", e, self.count[e]) for e in ENGS if e != "sp" and self.count[e] > 0]
        toks += [("d", i, v) for i, v in enumerate(self.dval) if v > 0]
        for e in ENGS:
            for t in toks:
                self._wait(e, t)

    def emit(self):
        nc = self.nc
        with nc.Block() as block:
            def run(engname, e):
                for item in self.streams[engname]:
                    if item[0] == "wait":
                        _, key, n = item
                        sem = self.esem[key[1]] if key[0] == "e" else self.dsem[key[1]]
                        e.wait_ge(sem, n)
                    elif item[0] == "op":
                        ins = item[1](e)
                        if item[2]:
                            ins.then_inc(self.esem[engname], 1)
                    else:
                        _, out, in_, idx, kw = item
                        e.dma_start(out=out, in_=in_, **kw).then_inc(self.dsem[idx], 16)

            @block.tensor
            def _(e):
                run("pe", e)

            @block.scalar
            def _(e):
                run("act", e)

            @block.vector
            def _(e):
                run("dve", e)

            @block.gpsimd
            def _(e):
                run("pool", e)

            @block.sync
            def _(e):
                run("sp", e)


class Ring:
    def __init__(self, bufs):
        self.bufs = list(bufs)
        self.free = [[] for _ in self.bufs]
        self.i = 0

    def get(self):
        i = self.i
        self.i = (i + 1) % len(self.bufs)
        deps = self.free[i]
        self.free[i] = []
        return i, self.bufs[i], list(deps)

    def release(self, i, toks):
        self.free[i] = [t for t in toks if t is not None]


class K:
    def __init__(self, nc, es):
        self.nc = nc
        self.P = Prog(nc, es)
        self.es = es

    def mm(self, out, lhsT, rhs, start, stop, deps=(), sig=None):
        if sig is None:
            sig = stop
        return self.P.op("pe", lambda e: e.matmul(out, lhsT=lhsT, rhs=rhs, start=start, stop=stop), deps, sig)

    def tr(self, out, in_, ident, deps=(), sig=True):
        return self.P.op("pe", lambda e: e.transpose(out, in_, ident), deps, sig)

    def act(self, out, in_, func, deps=(), **kw):
        return self.P.op("act", lambda e: e.activation(out=out, in_=in_, func=func, **kw), deps)

    def ts(self, eng, out, in0, s1, s2, op0, op1=None, deps=()):
        if op1 is None:
            return self.P.op(eng, lambda e: e.tensor_scalar(out=out, in0=in0, scalar1=s1, scalar2=None, op0=op0), deps)
        return self.P.op(eng, lambda e: e.tensor_scalar(out=out, in0=in0, scalar1=s1, scalar2=s2, op0=op0, op1=op1), deps)

    def stt(self, eng, out, in0, scalar, in1, op0, op1, deps=()):
        return self.P.op(eng, lambda e: e.scalar_tensor_tensor(out=out, in0=in0, scalar=scalar, in1=in1, op0=op0, op1=op1), deps)

    def tt(self, eng, out, in0, in1, op, deps=()):
        return self.P.op(eng, lambda e: e.tensor_tensor(out=out, in0=in0, in1=in1, op=op), deps)

    def cp(self, eng, out, in_, deps=()):
        if eng == "act":
            return self.P.op("act", lambda e: e.activation(out=out, in_=in_, func=AF.Copy), deps)
        return self.P.op(eng, lambda e: e.tensor_copy(out=out, in_=in_), deps)

    def recip(self, out, in_, deps=()):
        return self.P.op("dve", lambda e: e.reciprocal(out=out, in_=in_), deps)

    def memset(self, eng, ap, val, deps=()):
        return self.P.op(eng, lambda e: e.memset(ap, val), deps)

    def dma(self, eng, out, in_, deps=()):
        return self.P.dma(eng, out, in_, deps)

    def _uniq(self, name):
        self._nctr = getattr(self, "_nctr", 0) + 1
        return "%s_%d" % (name, self._nctr)

    def sb(self, es, name, shape, dt):
        return es.enter_context(self.nc.sbuf_tensor(self._uniq(name), shape, dt))

    def ps(self, es, name, shape, dt=F32):
        return es.enter_context(self.nc.psum_tensor(self._uniq(name), shape, dt))

    def norm_group(self, R, xs, A, SH, j, dst, c0):
        n = len(xs)
        si, ssq, sdeps = R["ssq"].get()
        tsq = []
        for t, (xt, deps) in enumerate(xs):
            tsq.append(self.act(R["junk"][:], xt, AF.Square, deps=list(deps) + sdeps, accum_out=ssq[:, t:t + 1]))
        tq = self.act(ssq[:, 4:4 + n], ssq[:, 0:n], AF.Sqrt, deps=tsq, scale=1.0 / D, bias=self.eps[:])
        trc = self.recip(ssq[:, 8:8 + n], ssq[:, 4:4 + n], deps=[tq])
        pi, pT, pdeps = R["pT"].get()
        ttr = []
        xn_rel = []
        for t, (xt, deps) in enumerate(xs):
            xi, xnb, xdeps = R["xnb"].get()
            tx = self.act(xnb[:], xt, AF.Copy, deps=[trc] + xdeps, scale=ssq[:, 8 + t:9 + t])
            last = None
            for k in range(8):
                last = self.tr(pT[:, k, t * 128:(t + 1) * 128], xnb[:, k * 128:(k + 1) * 128], self.identb[:],
                               deps=([tx] + pdeps) if k == 0 else (), sig=(k == 7))
            R["xnb"].release(xi, [last])
            ttr.append(last)
        R["ssq"].release(si, [tx])
        tev = []
        for k in range(8):
            o = dst[:, k, c0:c0 + n * 128]
            i_ = pT[:, k, 0:n * 128]
            tev.append(self.ts("dve", o, i_, A[:, k, j:j + 1], SH[:, k, j:j + 1], ALU.mult, ALU.add, deps=ttr))
        R["pT"].release(pi, tev[-2:])
        return tev[-2:], tsq

    def gating(self, R, lg_ps, tile, Gall, rb, deps):
        i, g, gdeps = R["gate"].get()
        lgs = g[:, 0:32]
        m8 = g[:, 32:40]
        negm = g[:, 40:41]
        mask = g[:, 48:80]
        ex = g[:, 80:112]
        ssum = g[:, 112:113]
        t1 = self.tt("dve", lgs, lg_ps, rb[:], ALU.add, deps=list(deps) + gdeps)
        t2 = self.P.op("dve", lambda e: e.max(out=m8, in_=lgs), [t1])
        t3 = self.ts("dve", mask, lgs, m8[:, 3:4], None, ALU.is_ge, deps=[t2])
        t4 = self.ts("dve", negm, m8[:, 0:1], -1.0, None, ALU.mult, deps=[t3])
        t5 = self.act(ex, lgs, AF.Exp, deps=[t4], bias=negm, scale=1.0)
        t6 = self.tt("dve", ex, ex, mask, ALU.mult, deps=[t5])
        t7 = self.P.op("dve", lambda e: e.reduce_sum(out=ssum, in_=ex, axis=AX.X), [t6])
        t8 = self.recip(ssum, ssum, deps=[t7])
        t9 = self.ts("dve", Gall[:, tile, :], ex, ssum, None, ALU.mult, deps=[t8])
        R["gate"].release(i, [t9])
        return t1, t9


def fm(v):
    v = np.asarray(v)
    return np.ascontiguousarray(v.reshape(-1, 128).T)


def phase_setup(k, I):
    nc, P = k.nc, k.P
    es = k.es
    k.identb = k.sb(es, "identb", [128, 128], BF16)
    k.identf = k.sb(es, "identf", [128, 128], F32)
    k.onesb = k.sb(es, "onesb", [128, 128], BF16)
    k.eps = k.sb(es, "eps", [128, 1], F32)
    k.scfm = k.sb(es, "scfm", [128, 8, 2], F32)
    k.Gall = k.sb(es, "Gall", [128, NT, NE], F32)
    k.dma("sp", k.identb[:], I["c_identb"])
    k.dma("sp", k.identf[:], I["c_identf"])
    k.dma("sp", k.scfm[:], I["cfm"])
    k.memset("dve", k.onesb[:], 1.0)
    k.memset("dve", k.eps[:], EPS)
    P.barrier()
    k.act(k.scfm[:], k.scfm[:], AF.Silu)
    P.barrier()


def phase_adaln(k, I, l):
    nc, P = k.nc, k.P
    es = k.es
    modT = k.sb(es, "modT%d" % l, [128, 48, 2], F32)
    A1 = k.sb(es, "A1_%d" % l, [128, 8, 2], F32)
    A2 = k.sb(es, "A2_%d" % l, [128, 8, 2], F32)
    gd = nc.dram_tensor("gd%d" % l, [2, 2048], F32, kind="Internal").ap()
    modw = I["l%d_mod_w" % l]
    with ExitStack() as s:
        wp = Ring([k.sb(s, "ad_wp%d" % i, [128, 8, 1024], F32) for i in range(2)])
        psfm = k.ps(s, "ad_psfm", [128, 48, 2])
        pstm = k.ps(s, "ad_pstm", [2, 2048])
        brow = k.sb(s, "ad_brow", [2, 2048], F32)
        gts = k.sb(s, "ad_gts", [2, 2048], F32)
        mb = k.sb(s, "ad_mb", [128, 48], F32)
        ng = k.sb(s, "ad_ng", [128, 16], F32)
        tmp = k.sb(s, "ad_tmp", [128, 16, 2], F32)
        d0 = [k.dma("sp", mb[:], I["l%d_mod_b_fm" % l]),
              k.dma("sp", ng[:, 0:8], I["l%d_norm1_g_fm" % l]),
              k.dma("sp", ng[:, 8:16], I["l%d_norm2_g_fm" % l])]
        mbrow = I["l%d_mod_b" % l]
        d0.append(k.dma("sp", brow[:, 0:1024], mbrow[:, 2048:3072].to_broadcast([2, 1024])))
        d0.append(k.dma("sp", brow[:, 1024:2048], mbrow[:, 5120:6144].to_broadcast([2, 1024])))
        tl = None
        for pc in range(6):
            i, buf, wdeps = wp.get()
            dts = [k.dma("sp", buf[:, kk, :], modw[kk * 128:(kk + 1) * 128, pc * 1024:(pc + 1) * 1024], deps=wdeps)
                   for kk in range(8)]
            t = None
            for jj in range(8):
                j = pc * 8 + jj
                for kk in range(8):
                    t = k.mm(psfm[:, j, :], buf[:, kk, jj * 128:(jj + 1) * 128], k.scfm[:, kk, :], kk == 0, kk == 7,
                             deps=dts if (jj == 0 and kk == 0) else ())
            if pc in (2, 5):
                gi = 0 if pc == 2 else 1
                for half in range(2):
                    for kk in range(8):
                        t = k.mm(pstm[:, gi * 1024 + half * 512:gi * 1024 + (half + 1) * 512], k.scfm[:, kk, :],
                                 buf[:, kk, half * 512:(half + 1) * 512], kk == 0, kk == 7)
            wp.release(i, [t])
            tl = t
        e1 = k.tt("dve", modT[:, :, 0], psfm[:, :, 0], mb[:], ALU.add, deps=[tl] + d0)
        e2 = k.tt("dve", modT[:, :, 1], psfm[:, :, 1], mb[:], ALU.add, deps=[tl])
        e3 = k.tt("dve", gts[:], pstm[:], brow[:], ALU.add, deps=[tl])
        dg = k.dma("sp", gd, gts[:], deps=[e3])
        e4 = k.ts("dve", tmp[:, 0:8, :], modT[:, 8:16, :], 1.0, None, ALU.add, deps=[e1, e2])
        e5 = k.ts("dve", tmp[:, 8:16, :], modT[:, 32:40, :], 1.0, None, ALU.add, deps=[e4])
        for j in range(2):
            k.tt("dve", A1[:, :, j], tmp[:, 0:8, j], ng[:, 0:8], ALU.mult, deps=[e5])
            k.tt("dve", A2[:, :, j], tmp[:, 8:16, j], ng[:, 8:16], ALU.mult, deps=[e5])
        P.barrier()
    k.mod[l] = dict(modT=modT, A1=A1, A2=A2, gd=gd)


def load_gate_rows(k, s, l, which, name):
    gb = k.sb(s, name, [128, 2, 1024], F32)
    gd = k.mod[l]["gd"]
    for j in range(2):
        k.dma("sp", gb[:, j, :], gd[j:j + 1, which * 1024:(which + 1) * 1024].to_broadcast([128, 1024]))
    return gb


def mk_norm_rings(k, s, pfx):
    R = {}
    R["ssq"] = Ring([k.sb(s, pfx + "ssq%d" % i, [128, 12], F32) for i in range(2)])
    R["junk"] = k.sb(s, pfx + "junk", [128, 1024], BF16)
    R["xnb"] = Ring([k.sb(s, pfx + "xnb%d" % i, [128, 1024], BF16) for i in range(4)])
    R["pT"] = Ring([k.ps(s, pfx + "pT%d" % i, [128, 8, 512], BF16) for i in range(1)])
    return R


def phase_l0_inproj(k, I, cqn, ckvn, krT, UT, cosT, sinT):
    nc, P = k.nc, k.P
    m = k.mod[0]
    with ExitStack() as s:
        win = k.sb(s, "win", [128, 8, 1024], BF16)
        gfm = k.sb(s, "gfm", [128, 5], F32)
        R = mk_norm_rings(k, s, "ip_")
        xt = Ring([k.sb(s, "ip_xt%d" % i, [128, 1024], F32) for i in range(4)])
        hTb = Ring([k.sb(s, "ip_hT%d" % i, [128, 8, 512], BF16) for i in range(2)])
        sq = k.sb(s, "ip_sq", [128, 3, 512], BF16)
        r32 = k.sb(s, "ip_r32", [128, 2, 512], F32)
        t12 = k.sb(s, "ip_t12", [64, 2, 512], F32)
        banks = Ring([k.ps(s, "ip_b%d" % i, [128, 512]) for i in range(4)])
        w_in = I["l0_w_in"]
        for kk in range(8):
            k.dma("pool", win[:, kk, 0:704], w_in[kk * 128:(kk + 1) * 128, 0:704])
            k.dma("pool", win[:, kk, 768:1024], w_in[kk * 128:(kk + 1) * 128, 704:960])
        k.dma("sp", gfm[:, 0:3], I["l0_q_norm_g_fm"])
        k.dma("sp", gfm[:, 3:5], I["l0_kv_norm_g_fm"])
        P.barrier()
        k.ts("dve", win[:, :, 704:720], win[:, :, 656:672], -1.0, None, ALU.mult)
        k.cp("dve", win[:, :, 720:736], win[:, :, 640:656])
        k.ts("dve", win[:, :, 736:752], win[:, :, 688:704], -1.0, None, ALU.mult)
        k.cp("dve", win[:, :, 752:768], win[:, :, 672:688])
        P.barrier()
        xin = I["xin"]
        if SUB == 1:
            return
        for bi, (s0, n) in enumerate(BLOCKS):
            if SUB < 99 and bi > 0:
                break
            j = 1 if bi == 0 else 0
            xs = []
            xrel = []
            for t in range(n // 128):
                xi, xb, xdeps = xt.get()
                d = k.dma("sp", xb[:], xin[s0 + t * 128:s0 + (t + 1) * 128, :], deps=xdeps)
                xs.append((xb[:], [d]))
                xrel.append(xi)
            hi, hb, hdeps = hTb.get()
            R["pT"].free[0] = R["pT"].free[0] + hdeps
            tev, tsq = k.norm_group(R, xs, m["A1"], m["modT"], j, hb, 0)
            for xi in xrel:
                xt.release(xi, tev)
            if SUB == 2:
                break

            def proj(c0, M, first_deps):
                bi_, bank, bdeps = banks.get()
                t = None
                for kk in range(8):
                    t = k.mm(bank[0:M, 0:n], win[:, kk, c0:c0 + M], hb[:, kk, 0:n], kk == 0, kk == 7,
                             deps=(first_deps + bdeps) if kk == 0 else ())
                return bi_, bank, t

            def rms_chunks(chunks, dstT, gcol, inv_n, r32s):
                tsqs = []
                for c, (b_i, bank, tk) in enumerate(chunks):
                    tsqs.append(k.act(sq[:, c, 0:n], bank[:, 0:n], AF.Square, deps=[tk]))
                pi, pb, pdeps = banks.get()
                t = None
                for c in range(len(chunks)):
                    t = k.mm(pb[:, 0:n], k.onesb[:], sq[:, c, 0:n], c == 0, c == len(chunks) - 1,
                             deps=(tsqs + pdeps) if c == 0 else ())
                ta = k.act(r32s[:, 0:n], pb[:, 0:n], AF.Sqrt, deps=[t], scale=inv_n, bias=k.eps[:])
                tb = k.recip(r32s[:, 0:n], r32s[:, 0:n], deps=[ta])
                banks.release(pi, [ta])
                outs = []
                for c, (b_i, bank, tk) in enumerate(chunks):
                    to = k.stt("dve", dstT[:, c, s0:s0 + n], bank[:, 0:n], gfm[:, gcol + c:gcol + c + 1], r32s[:, 0:n],
                               ALU.mult, ALU.mult, deps=[tb])
                    banks.release(b_i, [to])
                    outs.append(to)
                return outs

            qch = [proj(c * 128, 128, tev if c == 0 else []) for c in range(3)]
            rms_chunks(qch, cqn, 0, 1.0 / 384, r32[:, 0, :])
            if SUB == 3:
                break
            kvch = [proj(384 + c * 128, 128, []) for c in range(2)]
            tkv = rms_chunks(kvch, ckvn, 3, 1.0 / 256, r32[:, 1, :])
            ai, abank, ta = proj(640, 64, [])
            bi2, bbank, tb = proj(704, 64, [])
            t1 = k.tt("dve", t12[:, 0, 0:n], abank[0:64, 0:n], cosT[:, s0:s0 + n], ALU.mult, deps=[ta])
            t2 = k.tt("dve", t12[:, 1, 0:n], bbank[0:64, 0:n], sinT[:, s0:s0 + n], ALU.mult, deps=[tb])
            t3 = k.tt("dve", krT[:, s0:s0 + n], t12[:, 0, 0:n], t12[:, 1, 0:n], ALU.add, deps=[t2])
            banks.release(ai, [t1])
            banks.release(bi2, [t2])
            tlast = None
            for c in range(2):
                ui, ubank, tu = proj(768 + c * 128, 128, [])
                tc = k.cp("act", UT[:, c, s0:s0 + n], ubank[:, 0:n], deps=[tu])
                banks.release(ui, [tc])
                tlast = tu
            hTb.release(hi, [tlast])
        P.barrier()


def phase_l0_fourier(k, I, UT, mix_d):
    nc, P = k.nc, k.P
    with ExitStack() as s:
        Z = k.sb(s, "f_Z", [128, NT, 512], BF16)
        CS = k.sb(s, "f_CS", [128, 256], BF16)
        d256 = k.sb(s, "f_d256", [128, 2, 2, 256], BF16)
        dbuf = Ring([k.sb(s, "f_db%d" % i, [128, 8, 2, 512], BF16) for i in range(2)])
        fst = Ring([k.sb(s, "f_st%d" % i, [128, 2, 512], BF16) for i in range(2)])
        banks = Ring([k.ps(s, "f_b%d" % i, [128, 512]) for i in range(4)])
        acc = Ring([[k.ps(s, "f_a%d_%d" % (i, c), [128, 512]) for c in range(2)] for i in range(2)])
        k.dma("sp", CS[:], I["c_cs64"])
        for cs_ in range(2):
            k.dma("sp", d256[:, :, cs_, :], I["c_dft256"][:, cs_, :].rearrange("(nt p) k -> p nt k", p=128))
        P.barrier()
        zt = []
        for i in range(NT):
            b_i, bank, bdeps = banks.get()
            t = None
            for c in range(2):
                t = k.mm(bank[:, c * 256:(c + 1) * 256], UT[:, c, i * 128:(i + 1) * 128], CS[:], True, True,
                         deps=bdeps if c == 0 else (), sig=(c == 1))
            te = k.cp("act" if i % 2 else "dve", Z[:, i, :], bank[:], deps=[t])
            banks.release(b_i, [te])
            zt.append(te)
        P.barrier()

        def evac(a_i, ab, tlast, s0, n):
            f_i, fb, fdeps = fst.get()
            t0 = k.cp("act", fb[:, 0, 0:n], ab[0][:, 0:n], deps=[tlast] + fdeps)
            t1 = k.cp("dve", fb[:, 1, 0:n], ab[1][:, 0:n], deps=[tlast] + fdeps)
            acc.release(a_i, [t0, t1])
            d = k.dma("pool", mix_d[6:8, :, s0:s0 + n].rearrange("c p t -> p c t"), fb[:, :, 0:n], deps=[t0, t1])
            fst.release(f_i, [d])

        a_i, ab, adeps = acc.get()
        t = None
        for nt in range(2):
            for c in range(2):
                k.mm(ab[c][:, 0:256], Z[:, nt, c * 256:c * 256 + 128], d256[:, nt, 0, :], nt == 0, False,
                     deps=adeps if (nt == 0 and c == 0) else (), sig=False)
                t = k.mm(ab[c][:, 0:256], Z[:, nt, c * 256 + 128:c * 256 + 256], d256[:, nt, 1, :], False, nt == 1)
        evac(a_i, ab, t, 0, 256)
        dft = I["c_dft4096"]
        for kb in range(8):
            a_i, ab, adeps = acc.get()
            t = None
            for pc in range(4):
                d_i, db, ddeps = dbuf.get()
                dd0 = k.dma("sp", db[:, :, 0, :], dft[pc * 1024:(pc + 1) * 1024, 0, kb * 512:(kb + 1) * 512]
                            .rearrange("(nt p) k -> p nt k", p=128), deps=ddeps)
                dd = k.dma("sp", db[:, :, 1, :], dft[pc * 1024:(pc + 1) * 1024, 1, kb * 512:(kb + 1) * 512]
                           .rearrange("(nt p) k -> p nt k", p=128), deps=ddeps)
                for nt in range(8):
                    zi = 2 + pc * 8 + nt
                    first = (pc == 0 and nt == 0)
                    last = (pc == 3 and nt == 7)
                    for c in range(2):
                        k.mm(ab[c][:], Z[:, zi, c * 256:c * 256 + 128], db[:, nt, 0, :], first, False,
                             deps=([dd0, dd] + (adeps if first else [])) if (nt == 0 and c == 0) else (), sig=False)
                        t = k.mm(ab[c][:], Z[:, zi, c * 256 + 128:c * 256 + 256], db[:, nt, 1, :], False, last,
                                 sig=(c == 1 and (last or nt == 7)))
                dbuf.release(d_i, [t])
            evac(a_i, ab, t, 256 + kb * 512, 512)
        P.barrier()


def phase_l0_attn(k, I, cqn, ckvn, krT, cosT, sinT, mix_d):
    nc, P = k.nc, k.P
    with ExitStack() as s:
        wq = k.sb(s, "a_wq", [128, 3, 6, 256], BF16)
        wkv = k.sb(s, "a_wkv", [128, 2, 6, 256], BF16)
        qn = [k.sb(s, "a_qn%d" % i, [128, T], BF16) for i in range(2)]
        qr = [k.sb(s, "a_qr%d" % i, [64, T], BF16) for i in range(2)]
        kn = [k.sb(s, "a_kn%d" % i, [128, T], BF16) for i in range(2)]
        vv = [k.sb(s, "a_vv%d" % i, [128, NT, 128], BF16) for i in range(2)]
        t12 = k.sb(s, "a_t12", [64, 2, 512], F32)
        pTr = Ring([k.sb(s, "a_pT%d" % i, [128, 512], BF16) for i in range(4)])
        rden = Ring([k.sb(s, "a_rd%d" % i, [128, 512], F32) for i in range(2)])
        ost = Ring([k.sb(s, "a_os%d" % i, [128, 512], BF16) for i in range(2)])
        banks = Ring([k.ps(s, "a_b%d" % i, [128, 512]) for i in range(4)])
        oacc = Ring([k.ps(s, "a_o%d" % i, [128, 512]) for i in range(2)])
        dacc = Ring([k.ps(s, "a_d%d" % i, [128, 512]) for i in range(2)])
        wqu = I["l0_w_q_up"]
        wkvu = I["l0_w_kv_up"]
        for c in range(3):
            k.dma("pool", wq[:, c, :, 0:192], wqu[c * 128:(c + 1) * 128, :].rearrange("p (h j) -> p h j", j=192))
        for c in range(2):
            k.dma("pool", wkv[:, c, :, :], wkvu[c * 128:(c + 1) * 128, :].rearrange("p (h j) -> p h j", j=256))
        P.barrier()
        for c in range(3):
            k.ts("dve", wq[:, c, :, 192:208], wq[:, c, :, 144:160], -1.0, None, ALU.mult)
            k.cp("dve", wq[:, c, :, 208:224], wq[:, c, :, 128:144])
            k.ts("dve", wq[:, c, :, 224:240], wq[:, c, :, 176:192], -1.0, None, ALU.mult)
            k.cp("dve", wq[:, c, :, 240:256], wq[:, c, :, 160:176])
        P.barrier()

        war = [[], []]

        def proj(h):
            b = h % 2
            toks = []
            for bi, (s0, n) in enumerate(BLOCKS):
                def grp(lhs_list, rhs_src, M):
                    b_i, bank, bdeps = banks.get()
                    t = None
                    for c, lhsT in enumerate(lhs_list):
                        t = k.mm(bank[0:M, 0:n], lhsT, rhs_src[:, c, s0:s0 + n], c == 0, c == len(lhs_list) - 1,
                                 deps=bdeps if c == 0 else ())
                    return b_i, bank, t
                wdeps = war[b] if bi == 0 else []
                b_i, bank, t = grp([wq[:, c, h, 0:128] for c in range(3)], cqn, 128)
                te = k.cp("act", qn[b][:, s0:s0 + n], bank[:, 0:n], deps=[t] + wdeps)
                banks.release(b_i, [te]); toks.append(te)
                b_i, bank, t = grp([wkv[:, c, h, 0:128] for c in range(2)], ckvn, 128)
                te = k.cp("dve", kn[b][:, s0:s0 + n], bank[:, 0:n], deps=[t] + wdeps)
                banks.release(b_i, [te]); toks.append(te)
                a_i, abank, ta = grp([wq[:, c, h, 128:192] for c in range(3)], cqn, 64)
                b2_i, bbank, tb = grp([wq[:, c, h, 192:256] for c in range(3)], cqn, 64)
                t1 = k.tt("dve", t12[:, 0, 0:n], abank[0:64, 0:n], cosT[:, s0:s0 + n], ALU.mult, deps=[ta])
                t2 = k.tt("dve", t12[:, 1, 0:n], bbank[0:64, 0:n], sinT[:, s0:s0 + n], ALU.mult, deps=[tb])
                t3 = k.tt("dve", qr[b][:, s0:s0 + n], t12[:, 0, 0:n], t12[:, 1, 0:n], ALU.add, deps=[t2] + wdeps)
                banks.release(a_i, [t1]); banks.release(b2_i, [t2]); toks.append(t3)
                nt = n // 128
                b_i, bank, bdeps = banks.get()
                t = None
                for tt_ in range(nt):
                    ti = s0 // 128 + tt_
                    for c in range(2):
                        t = k.mm(bank[:, tt_ * 128:(tt_ + 1) * 128], ckvn[:, c, ti * 128:(ti + 1) * 128], wkv[:, c, h, 128:256],
                                 c == 0, c == 1, deps=bdeps if (tt_ == 0 and c == 0) else (), sig=(c == 1 and tt_ == nt - 1))
                te = k.cp("act", vv[b][:, s0 // 128:s0 // 128 + nt, :], bank[:, 0:n].rearrange("p (t d) -> p t d", d=128),
                          deps=[t] + wdeps)
                banks.release(b_i, [te]); toks.append(te)
            return toks

        def attn(h, ptoks):
            b = h % 2
            lastpe = None
            for bi, (s0, n) in enumerate(BLOCKS):
                ktiles = [0, 1] if bi == 0 else list(range(NT))
                o_i, ob, odeps = oacc.get()
                d_i, db, ddeps = dacc.get()
                pend = None
                nk = len(ktiles)
                for idx in range(nk + 1):
                    cur = None
                    if idx < nk:
                        kt = ktiles[idx]
                        s_i, sbk, sdeps = banks.get()
                        k.mm(sbk[:, 0:n], kn[b][:, kt * 128:(kt + 1) * 128], qn[b][:, s0:s0 + n], True, False,
                             deps=sdeps + (ptoks if (bi == 0 and idx == 0) else []), sig=False)
                        t = k.mm(sbk[:, 0:n], krT[:, kt * 128:(kt + 1) * 128], qr[b][:, s0:s0 + n], False, True)
                        p_i, pb, pdeps = pTr.get()
                        te = k.act(pb[:, 0:n], sbk[:, 0:n], AF.Exp, deps=[t] + pdeps, scale=ATT_SCALE)
                        banks.release(s_i, [te])
                        cur = (kt, p_i, pb, te, idx)
                    if pend is not None:
                        kt, p_i, pb, te, ii = pend
                        k.mm(ob[:, 0:n], vv[b][:, kt, :], pb[:, 0:n], ii == 0, ii == nk - 1,
                             deps=[te] + (odeps + ddeps if ii == 0 else []), sig=False)
                        t2 = k.mm(db[:, 0:n], k.onesb[:], pb[:, 0:n], ii == 0, ii == nk - 1, sig=True)
                        pTr.release(p_i, [t2])
                        lastpe = t2
                    pend = cur
                r_i, rb, rdeps = rden.get()
                trr = k.recip(rb[:, 0:n], db[:, 0:n], deps=[lastpe] + rdeps)
                dacc.release(d_i, [trr])
                os_i, osb, osdeps = ost.get()
                to = k.tt("dve", osb[:, 0:n], ob[:, 0:n], rb[:, 0:n], ALU.mult, deps=[trr] + osdeps)
                oacc.release(o_i, [to])
                rden.release(r_i, [to])
                dd = k.dma("sp", mix_d[h, :, s0:s0 + n], osb[:, 0:n], deps=[to])
                ost.release(os_i, [dd])
            war[b] = [lastpe]

        pt = proj(0)
        for h in range(6):
            nxt = proj(h + 1) if h + 1 < 6 else None
            attn(h, pt)
            pt = nxt
        P.barrier()


def phase_wout(k, I, l, get_mix, xsrc, xres, h2T_d, tiles_range):
    nc, P = k.nc, k.P
    m = k.mod[l]
    with ExitStack() as s:
        wout = k.sb(s, "w_wout", [128, 8, 1024], BF16)
        rw = k.sb(s, "w_rw", [128, 8, NE], BF16)
        rb = k.sb(s, "w_rb", [128, NE], F32)
        g1b = load_gate_rows(k, s, l, 0, "w_g1b")
        R = mk_norm_rings(k, s, "w_")
        R["gate"] = Ring([k.sb(s, "w_gt%d" % i, [128, 128], F32) for i in range(2)])
        k.R_cur = R
        xt = Ring([k.sb(s, "w_xt%d" % i, [128, 1024], F32) for i in range(5)])
        tmp = Ring([k.sb(s, "w_tmp%d" % i, [128, 1024], F32) for i in range(2)])
        hTb = Ring([k.sb(s, "w_hT%d" % i, [128, 8, 512], BF16) for i in range(2)])
        ybank = [k.ps(s, "w_y%d" % i, [128, 512]) for i in range(2)]
        yring = Ring([ybank])
        lgb = Ring([k.ps(s, "w_lg%d" % i, [128, 512]) for i in range(2)])
        wo = I["l%d_w_out" % l]
        rwd = I["l%d_router_w" % l]
        for kk in range(8):
            k.dma("pool", wout[:, kk, :], wo[kk * 128:(kk + 1) * 128, :])
            k.dma("pool", rw[:, kk, :], rwd[kk * 128:(kk + 1) * 128, :])
        k.dma("sp", rb[:], I["l%d_router_b" % l].to_broadcast([128, NE]))
        P.barrier()
        for bi, (s0, n) in enumerate(BLOCKS):
            if s0 // 128 < tiles_range[0]:
                continue
            j = 1 if bi == 0 else 0
            mixb, mtoks, mrel = get_mix(bi, s0, n)
            xs = []
            xrel = []
            lastmm = None
            for t in range(n // 128):
                ti = s0 // 128 + t
                xi, xb, xdeps = xt.get()
                d = k.dma("sp", xb[:], xsrc[ti * 128:(ti + 1) * 128, :], deps=xdeps)
                y_i, yb, ydeps = yring.get()
                for half in range(2):
                    for c in range(8):
                        lastmm = k.mm(yb[half][:], mixb[:, c, t * 128:(t + 1) * 128], wout[:, c, half * 512:(half + 1) * 512],
                                      c == 0, c == 7, deps=(mtoks + ydeps) if (c == 0 and half == 0) else ())
                tm_i, tb, tdeps = tmp.get()
                t1 = k.tt("dve", tb[:, 0:512], yb[0][:], g1b[:, j, 0:512], ALU.mult, deps=[lastmm] + tdeps)
                t2 = k.tt("dve", tb[:, 512:1024], yb[1][:], g1b[:, j, 512:1024], ALU.mult, deps=[lastmm])
                yring.release(y_i, [t2])
                t3 = k.tt("pool", xb[:], tb[:], xb[:], ALU.add, deps=[t2, d])
                tmp.release(tm_i, [t3])
                dst = k.dma("pool", xres[ti * 128:(ti + 1) * 128, :], xb[:], deps=[t3])
                xs.append((xb[:], [t3]))
                xrel.append((xi, dst))
            mrel([lastmm])
            hi, hb, hdeps = hTb.get()
            R["pT"].free[0] = R["pT"].free[0] + hdeps
            tev, tsq = k.norm_group(R, xs, m["A2"], m["modT"][:, 24:32, :], j, hb, 0)
            for xi, dst in xrel:
                xt.release(xi, tev + [dst])
            dh = k.dma("pool", h2T_d[:, :, s0:s0 + n].rearrange("c p t -> p c t"), hb[:, :, 0:n], deps=tev)
            lt = None
            for t in range(n // 128):
                ti = s0 // 128 + t
                l_i, lb, ldeps = lgb.get()
                for c in range(8):
                    lt = k.mm(lb[:, 0:NE], hb[:, c, t * 128:(t + 1) * 128], rw[:, c, :], c == 0, c == 7,
                              deps=(tev + ldeps) if c == 0 else ())
                t1, t9 = k.gating(R, lb[:, 0:NE], ti, k.Gall, rb, [lt])
                lgb.release(l_i, [t1])
            hTb.release(hi, [lt, dh])
        P.barrier()


def phase_moe(k, I, l, xres, h2T_d, groups, final_out=None):
    nc, P = k.nc, k.P
    with ExitStack() as s:
        GMAX = max(g[1] - g[0] for g in groups)
        hTg = k.sb(s, "m_hT", [128, 8, GMAX * 128], BF16)
        yacc = k.sb(s, "m_yacc", [128, GMAX, 1024], F32)
        wgu = Ring([k.sb(s, "m_wgu%d" % i, [128, 8, 2048], BF16) for i in range(2)])
        wd = Ring([k.sb(s, "m_wd%d" % i, [128, 8, 1024], BF16) for i in range(2)])
        aT = Ring([k.sb(s, "m_aT%d" % i, [128, 8, 512], BF16) for i in range(2)])
        tg = Ring([k.sb(s, "m_tg%d" % i, [128, 4, 512], F32) for i in range(1)])
        bgu = k.sb(s, "m_bgu", [128, NE, 8, 2], F32)
        bdn = k.sb(s, "m_bdn", [NE, 1024], F32)
        GTs = Ring([k.sb(s, "m_GT%d" % i, [NE, 128], F32) for i in range(2)])
        g2b = load_gate_rows(k, s, l, 1, "m_g2b")
        xt = Ring([k.sb(s, "m_xt%d" % i, [128, 1024], F32) for i in range(2)])
        glb = Ring([[k.ps(s, "m_gl%d_%d" % (i, c), [128, 512]) for c in range(2)] for i in range(2)])
        yb = Ring([[k.ps(s, "m_y%d_%d" % (i, c), [128, 512]) for c in range(2)] for i in range(2)])
        fng = None
        if final_out is not None:
            fng = k.sb(s, "m_fng", [128, 1024], F32)
            k.dma("sp", fng[:], I["final_norm_g"].to_broadcast([128, 1024]))
            fsq = k.sb(s, "m_fsq", [128, 4], F32)
            fjunk = k.sb(s, "m_fjunk", [128, 1024], BF16)
        k.dma("sp", bgu[:], I["l%d_b_gu_fm" % l])
        k.dma("sp", bdn[:], I["l%d_b_down" % l])
        P.barrier()
        k.ts("dve", bgu[:, :, :, 1], bgu[:, :, :, 1], 1.0, None, ALU.add)
        P.barrier()
        w_gu = I["l%d_w_gu" % l]
        w_dn = I["l%d_w_down" % l]

        def load_w(e, first=False):
            gi, gbuf, gdeps = wgu.get()
            di, dbuf, ddeps = wd.get()
            tk = []
            for kk in range(8):
                tk.append(k.dma("pool", gbuf[:, kk, :], w_gu[e, kk * 128:(kk + 1) * 128, :], deps=gdeps if kk == 0 else ()))
            for kk in range(8):
                tk.append(k.dma("pool", dbuf[:, kk, :], w_dn[e, kk * 128:(kk + 1) * 128, :], deps=ddeps if kk == 0 else ()))
            return (gi, gbuf, di, dbuf, tk)

        hT_last = []
        fin_last = []
        for gidx, (ta, tb_) in enumerate(groups):
            ng = tb_ - ta
            c0g = ta * 128
            dh = k.dma("sp", hTg[:, :, 0:ng * 128], h2T_d[:, :, c0g:c0g + ng * 128].rearrange("c p t -> p c t"), deps=hT_last)
            nxt = load_w(0)
            tinit = []
            for ti in range(ng):
                g_i, gp, gdeps = glb.get()
                ttr = k.tr(gp[0][0:NE, 0:128], k.Gall[:, ta + ti, :], k.identf[:], deps=gdeps)
                gt_i, gts, gtdeps = GTs.get()
                tc = k.cp("act", gts[:], gp[0][0:NE, 0:128], deps=[ttr] + gtdeps)
                glb.release(g_i, [tc])
                y_i, ybk, ydeps = yb.get()
                tm = None
                for half in range(2):
                    tm = k.mm(ybk[half][:], gts[:], bdn[:, half * 512:(half + 1) * 512], True, True,
                              deps=([tc] + ydeps) if half == 0 else ())
                GTs.release(gt_i, [tm])
                t0 = k.cp("act", yacc[:, ti, 0:512], ybk[0][:], deps=[tm] + fin_last)
                t1 = k.cp("dve", yacc[:, ti, 512:1024], ybk[1][:], deps=[tm] + fin_last)
                yb.release(y_i, [t0, t1])
                tinit += [t0, t1]
            tinit = tinit[-2:]
            subs = []
            t_ = 0
            if l == 0 and gidx == 0:
                subs.append((0, 2)); t_ = 2
            while t_ < ng:
                subs.append((t_, min(4, ng - t_))); t_ += min(4, ng - t_)
            yacc_tok = {}
            for e in range(NE):
                gi, gbuf, di, dbuf, wtok = nxt
                if e + 1 < NE:
                    nxt = load_w(e + 1)
                lastpe = None
                for (st, ntl) in subs:
                    n = ntl * 128
                    cc = st * 128
                    a_i, ab, adeps = aT.get()
                    for jf in range(8):
                        p_i, pr, pdeps = glb.get()
                        first = [dh] + wtok if (lastpe is None and jf == 0) else []
                        for kk in range(8):
                            k.mm(pr[0][:, 0:n], gbuf[:, kk, jf * 256:(jf + 1) * 256:2], hTg[:, kk, cc:cc + n], kk == 0, kk == 7,
                                 deps=(first + pdeps) if kk == 0 else (), sig=False)
                        tmm = None
                        for kk in range(8):
                            tmm = k.mm(pr[1][:, 0:n], gbuf[:, kk, jf * 256 + 1:(jf + 1) * 256:2], hTg[:, kk, cc:cc + n], kk == 0, kk == 7)
                        lastpe = tmm
                        tg_i, tgb, tgdeps = tg.get()
                        e1 = k.ts("dve", tgb[:, 0, 0:n], pr[0][:, 0:n], bgu[:, e, jf, 0:1], 7.0, ALU.add, ALU.min, deps=[tmm] + tgdeps)
                        e2 = k.act(tgb[:, 1, 0:n], tgb[:, 0, 0:n], AF.Sigmoid, deps=[e1], scale=1.702)
                        e3 = k.ts("dve", tgb[:, 2, 0:n], pr[1][:, 0:n], bgu[:, e, jf, 1:2], 8.0, ALU.add, ALU.min, deps=[tmm])
                        glb.release(p_i, [e3])
                        e4 = k.stt("dve", tgb[:, 3, 0:n], tgb[:, 2, 0:n], -6.0, tgb[:, 0, 0:n], ALU.max, ALU.mult, deps=[e3])
                        e5 = k.tt("dve", ab[:, jf, 0:n], tgb[:, 3, 0:n], tgb[:, 1, 0:n], ALU.mult, deps=[e4, e2] + (adeps if jf == 0 else []))
                        tg.release(tg_i, [e5])
                    for t in range(ntl):
                        ti = st + t
                        y_i, ybk, ydeps = yb.get()
                        tm = None
                        for half in range(2):
                            for jf in range(8):
                                tm = k.mm(ybk[half][:], ab[:, jf, t * 128:(t + 1) * 128], dbuf[:, jf, half * 512:(half + 1) * 512],
                                          jf == 0, jf == 7, deps=([e5] + ydeps) if (jf == 0 and half == 0) else ())
                        lastpe = tm
                        prev = yacc_tok.get(ti, tinit)
                        u0 = k.stt("dve", yacc[:, ti, 0:512], ybk[0][:], k.Gall[:, ta + ti, e:e + 1], yacc[:, ti, 0:512],
                                   ALU.mult, ALU.add, deps=[tm] + prev)
                        u1 = k.stt("dve", yacc[:, ti, 512:1024], ybk[1][:], k.Gall[:, ta + ti, e:e + 1], yacc[:, ti, 512:1024],
                                   ALU.mult, ALU.add, deps=[tm])
                        yb.release(y_i, [u1])
                        yacc_tok[ti] = [u1]
                    aT.release(a_i, [lastpe])
                wgu.release(gi, [lastpe])
                wd.release(di, [lastpe])
                hT_last = [lastpe]
            for ti in range(ng):
                tile = ta + ti
                j = 1 if tile < 2 else 0
                xi, xb, xdeps = xt.get()
                d = k.dma("sp", xb[:], xres[tile * 128:(tile + 1) * 128, :], deps=xdeps)
                f1 = k.tt("dve", yacc[:, ti, :], yacc[:, ti, :], g2b[:, j, :], ALU.mult, deps=yacc_tok[ti])
                f2 = k.tt("dve", xb[:], xb[:], yacc[:, ti, :], ALU.add, deps=[f1, d])
                fin_last = [f2]
                if final_out is None:
                    dd = k.dma("sp", xres[tile * 128:(tile + 1) * 128, :], xb[:], deps=[f2])
                else:
                    q1 = k.act(fjunk[:], xb[:], AF.Square, deps=[f2], accum_out=fsq[:, 0:1])
                    q2 = k.act(fsq[:, 1:2], fsq[:, 0:1], AF.Sqrt, deps=[q1], scale=1.0 / D, bias=k.eps[:])
                    q3 = k.recip(fsq[:, 2:3], fsq[:, 1:2], deps=[q2])
                    q4 = k.stt("dve", xb[:], xb[:], fsq[:, 2:3], fng[:], ALU.mult, ALU.mult, deps=[q3])
                    dd = k.dma("sp", final_out[(tile - 2) * 128:(tile - 1) * 128, :], xb[:], deps=[q4])
                xt.release(xi, [dd])
            P.barrier()


def _bf(a):
    return np.asarray(a, np.float32).astype(ml_dtypes.bfloat16)


def make_consts():
    C = {}
    C["c_identb"] = _bf(np.eye(128))
    C["c_identf"] = np.eye(128, dtype=np.float32)
    pos = np.arange(TL)
    row = (pos // 64).astype(np.float32)
    col = (pos % 64).astype(np.float32)
    inv = (np.float32(10000.0) ** (-np.arange(0, 32, 2, dtype=np.float32) / np.float32(32))).astype(np.float32)
    ar = row[:, None] * inv[None, :]
    ac = col[:, None] * inv[None, :]
    ang = np.concatenate([ar, ar, ac, ac], -1).astype(np.float32)
    cosT = np.ones((64, T), np.float32)
    sinT = np.zeros((64, T), np.float32)
    cosT[:, TC:] = np.cos(ang).T
    sinT[:, TC:] = np.sin(ang).T
    C["c_cos"] = cosT
    C["c_sin"] = sinT
    jl = np.outer(np.arange(64), np.arange(64)) % 64
    c64 = np.cos(2 * np.pi * jl / 64.0) / 8.0
    s64 = np.sin(2 * np.pi * jl / 64.0) / 8.0
    cs = np.zeros((128, 256))
    for g in range(2):
        cs[g * 64:(g + 1) * 64, g * 64:(g + 1) * 64] = c64
        cs[g * 64:(g + 1) * 64, 128 + g * 64:128 + (g + 1) * 64] = s64
    C["c_cs64"] = _bf(cs)

    def dft(N):
        m = (np.arange(N, dtype=np.int64)[:, None] * np.arange(N, dtype=np.int64)[None, :]) % N
        a = 2 * np.pi * m.astype(np.float64) / N
        out = np.empty((N, 2, N), dtype=ml_dtypes.bfloat16)
        out[:, 0, :] = (np.cos(a) / np.sqrt(N)).astype(np.float32).astype(ml_dtypes.bfloat16)
        out[:, 1, :] = (-np.sin(a) / np.sqrt(N)).astype(np.float32).astype(ml_dtypes.bfloat16)
        return out
    jj = np.arange(128)[:, None]
    ii = np.arange(128)[None, :]
    same = (jj // 64) == (ii // 64)
    tri = np.zeros((128, 4, 128), np.float32)
    tri[:, 0, :] = np.where(same & (jj <= ii), -1.0 / 16, 0.0)
    tri[:, 1, :] = np.where(same & (jj >= ii), -1.0 / 16, 0.0)
    tri[:, 2, :] = np.where(same & (jj > ii), -1.0 / 16, 0.0)
    tri[:, 3, :] = np.where(same & (jj < ii), -1.0 / 16, 0.0)
    C["c_tri"] = tri
    C["c_lnS"] = np.full((128, 1), np.log(128.0 ** -0.5), np.float32)
    j6 = np.arange(64)[:, None]
    i6 = np.arange(64)[None, :]
    mk = np.zeros((64, 2, 256), np.float32)
    for h in range(4):
        mk[:, 0, h * 64:(h + 1) * 64] = (j6 <= i6)
        mk[:, 1, h * 64:(h + 1) * 64] = (j6 >= i6)
    C["c_mask"] = _bf(mk)
    C["c_dft256"] = dft(256)
    C["c_dft4096"] = dft(4096)
    return C


_CONSTS = None

IN_SPECS = None


def input_specs():
    sp = {
        "xin": ([T, D], F32), "cfm": ([128, 8, 2], F32), "final_norm_g": ([1, D], F32),
        "c_identb": ([128, 128], BF16), "c_identf": ([128, 128], F32), "c_cos": ([64, T], F32), "c_sin": ([64, T], F32),
        "c_cs64": ([128, 256], BF16), "c_dft256": ([256, 2, 256], BF16), "c_dft4096": ([4096, 2, 4096], BF16),
    }
    for l in range(2):
        p = "l%d_" % l
        sp[p + "mod_w"] = ([D, 6 * D], F32)
        sp[p + "mod_b"] = ([1, 6 * D], F32)
        sp[p + "mod_b_fm"] = ([128, 48], F32)
        sp[p + "norm1_g_fm"] = ([128, 8], F32)
        sp[p + "norm2_g_fm"] = ([128, 8], F32)
        sp[p + "w_out"] = ([D, D], F32)
        sp[p + "router_w"] = ([D, NE], F32)
        sp[p + "router_b"] = ([1, NE], F32)
        sp[p + "w_gu"] = ([NE, D, 2 * D], F32)
        sp[p + "b_gu_fm"] = ([128, NE, 8, 2], F32)
        sp[p + "w_down"] = ([NE, D, D], F32)
        sp[p + "b_down"] = ([NE, D], F32)
    sp["l1_w_in"] = ([D, 3104], F32)
    sp["l1_wz"] = ([33, 1024], F32)
    sp["l1_gnorm_g"] = ([1, 256], F32)
    sp["c_tri"] = ([128, 4, 128], F32)
    sp["c_lnS"] = ([128, 1], F32)
    sp["c_mask"] = ([64, 2, 256], BF16)
    sp["l0_w_in"] = ([D, 960], F32)
    sp["l0_q_norm_g_fm"] = ([128, 3], F32)
    sp["l0_w_q_up"] = ([384, 1152], F32)
    sp["l0_kv_norm_g_fm"] = ([128, 2], F32)
    sp["l0_w_kv_up"] = ([256, 1536], F32)
    return sp


ALL_INPUT_NAMES = (
    "x", "c", "ctx", "c_ctx", "final_norm_g",
    "l0_mod_w", "l0_mod_b", "l0_norm1_g", "l0_w_in", "l0_q_norm_g", "l0_w_q_up", "l0_kv_norm_g", "l0_w_kv_up",
    "l0_w_out", "l0_norm2_g", "l0_router_w", "l0_router_b", "l0_w_gu", "l0_b_gu", "l0_w_down", "l0_b_down",
    "l1_mod_w", "l1_mod_b", "l1_norm1_g", "l1_w_in", "l1_w_gk_fwd", "l1_b_gk_fwd", "l1_w_gk_bwd", "l1_b_gk_bwd",
    "l1_gnorm_g", "l1_w_out", "l1_norm2_g", "l1_router_w", "l1_router_b", "l1_w_gu", "l1_b_gu", "l1_w_down",
    "l1_b_down",
)


def host_inputs(inputs, b):
    global _CONSTS
    if _CONSTS is None:
        _CONSTS = make_consts()
    missing = [n for n in ALL_INPUT_NAMES if n not in inputs]
    assert not missing, missing
    g = lambda n: np.asarray(inputs[n], np.float32)
    m = dict(_CONSTS)
    m["xin"] = np.ascontiguousarray(np.concatenate([g("ctx")[b], g("x")[b]], 0))
    m["cfm"] = np.ascontiguousarray(np.stack([fm(g("c")[b]), fm(g("c_ctx"))], -1))
    m["final_norm_g"] = g("final_norm_g").reshape(1, D)
    for l in range(2):
        p = "l%d_" % l
        m[p + "mod_w"] = g(p + "mod_w")
        m[p + "mod_b"] = g(p + "mod_b").reshape(1, -1)
        m[p + "mod_b_fm"] = fm(g(p + "mod_b"))
        m[p + "norm1_g_fm"] = fm(g(p + "norm1_g"))
        m[p + "norm2_g_fm"] = fm(g(p + "norm2_g"))
        m[p + "w_out"] = g(p + "w_out")
        m[p + "router_w"] = g(p + "router_w")
        m[p + "router_b"] = g(p + "router_b").reshape(1, NE)
        m[p + "w_gu"] = g(p + "w_gu")
        m[p + "b_gu_fm"] = np.ascontiguousarray(g(p + "b_gu").reshape(NE, 8, 128, 2).transpose(2, 0, 1, 3))
        m[p + "w_down"] = g(p + "w_down")
        m[p + "b_down"] = g(p + "b_down")
    m["l1_w_in"] = g("l1_w_in")
    wz = np.zeros((33, 1024), np.float32)
    wz[0:16, 0:512] = g("l1_w_gk_fwd")
    wz[16:32, 512:1024] = g("l1_w_gk_bwd")
    wz[32, 0:512] = g("l1_b_gk_fwd")
    wz[32, 512:1024] = g("l1_b_gk_bwd")
    m["l1_wz"] = wz
    m["l1_gnorm_g"] = g("l1_gnorm_g").reshape(1, 256)
    m["l0_w_in"] = g("l0_w_in")
    m["l0_q_norm_g_fm"] = fm(g("l0_q_norm_g"))
    m["l0_w_q_up"] = g("l0_w_q_up")
    m["l0_kv_norm_g_fm"] = fm(g("l0_kv_norm_g"))
    m["l0_w_kv_up"] = g("l0_w_kv_up")
    return m


L0_GROUPS = [(0, 6), (6, 13), (13, 20), (20, 27), (27, 34)]
L1_GROUPS = [(2, 9), (9, 16), (16, 22), (22, 28), (28, 34)]


def build():
    nc = bass.Bass("TRN2", target_bir_lowering=False)
    I = {}
    for name, (shape, dt) in input_specs().items():
        if L1_ONLY and (name.startswith("l0_") or name in ("c_dft4096", "c_dft256", "c_cs64", "c_cos", "c_sin", "xin")
                        or name in ("l1_w_gu", "l1_w_down", "l1_b_gu_fm", "l1_b_down")):
            continue
        I[name] = nc.dram_tensor(name, shape, dt, kind="ExternalInput").ap()
    if L1_ONLY:
        xres = nc.dram_tensor("xres_in", [T, D], F32, kind="ExternalInput").ap()
    else:
        out = nc.dram_tensor("out", [TL, D], F32, kind="ExternalOutput").ap()
        xres = nc.dram_tensor("xres", [T, D], F32, kind="Internal").ap()
    mix_d = nc.dram_tensor("mix_d", [8, 128, T], BF16, kind="Internal").ap()
    h2T_d = nc.dram_tensor("h2T_d", [8, 128, T], BF16, kind="Internal").ap()
    dbg = {}
    with ExitStack() as es:
        k = K(nc, es)
        k.mod = {}
        P = k.P
        stop = STOP_AFTER
        dbg_src = None

        def body():
            nonlocal dbg_src
            phase_setup(k, I)
            if stop == "setup":
                dbg_src = I["xin"]; return
            if not L1_ONLY:
                phase_adaln(k, I, 0)
            phase_adaln(k, I, 1)
            if stop == "adaln":
                dbg_src = I["xin"]; return
            if not L1_ONLY:
                body_l0()
                if dbg_src is not None:
                    return
            body_l1()

        def body_l0():
            nonlocal dbg_src
            with ExitStack() as s0:
                cosT = k.sb(s0, "cosT", [64, T], F32)
                sinT = k.sb(s0, "sinT", [64, T], F32)
                cqn = k.sb(s0, "cqn", [128, 3, T], BF16)
                ckvn = k.sb(s0, "ckvn", [128, 2, T], BF16)
                krT = k.sb(s0, "krT", [64, T], BF16)
                k.dma("sp", cosT[:], I["c_cos"])
                k.dma("sp", sinT[:], I["c_sin"])
                with ExitStack() as s1:
                    UT = k.sb(s1, "UT", [128, 2, T], BF16)
                    phase_l0_inproj(k, I, cqn, ckvn, krT, UT, cosT, sinT)
                    if stop == "inproj":
                        dbg_src = I["xin"]; return
                    phase_l0_fourier(k, I, UT, mix_d)
                    if stop == "fourier":
                        dbg_src = I["xin"]; return
                phase_l0_attn(k, I, cqn, ckvn, krT, cosT, sinT, mix_d)
                if stop == "attn":
                    dbg_src = I["xin"]; return
            with ExitStack() as s2:
                mixr = Ring([k.sb(s2, "mixb%d" % i, [128, 8, 512], BF16) for i in range(2)])

                def get_mix(bi, s0_, n):
                    i, buf, deps = mixr.get()
                    d = k.dma("sp", buf[:, :, 0:n], mix_d[:, :, s0_:s0_ + n].rearrange("c p t -> p c t"), deps=deps)
                    return buf, [d], (lambda toks, i=i: mixr.release(i, toks))
                phase_wout(k, I, 0, get_mix, I["xin"], xres, h2T_d, (0, NT))
            if stop == "l0_wout":
                dbg_src = xres; return
            phase_moe(k, I, 0, xres, h2T_d, L0_GROUPS)
            if stop == "l0":
                dbg_src = xres; return

        def body_l1():
            nonlocal dbg_src
            with ExitStack() as s3:
                S1 = dict(
                    QD=nc.dram_tensor("QD_d", [8, 128, T], BF16, kind="Internal").ap(),
                    KD=nc.dram_tensor("KD_d", [8, 128, T], BF16, kind="Internal").ap(),
                    KK=nc.dram_tensor("KK_d", [T, 2, 512], BF16, kind="Internal").ap(),
                    V=nc.dram_tensor("V_d", [T, 1024], BF16, kind="Internal").ap(),
                    GO=nc.dram_tensor("GO_d", [T, 1024], BF16, kind="Internal").ap(),
                    O=nc.dram_tensor("O_d", [2, TL, 1024], F32, kind="Internal").ap(),
                    EL=k.sb(s3, "EL", [128, 8, 68], F32))
                phase_l1_prep(k, I, xres, S1)
                phase_l1_scan(k, I, S1)
                if stop == "l1_scan":
                    dbg_src = S1["O"][0]; return
                with ExitStack() as s4:
                    gm = l1_get_mix_factory(k, I, s4, S1)
                    k.P.barrier()
                    phase_wout(k, I, 1, gm, xres, xres, h2T_d, (2, NT))
            if stop == "l1_wout":
                dbg_src = xres; return
            phase_moe(k, I, 1, xres, h2T_d, L1_GROUPS, final_out=out)

        body()
        if dbg_src is not None:
            nrows = dbg_src.shape[0]
            dbg_out = nc.dram_tensor("dbg", [nrows, D], F32, kind="ExternalOutput").ap()
            copy_dram(k, dbg_out, dbg_src, nrows // 128)
        P.barrier()
        P.emit()
    return nc


def copy_dram(k, dst, src, ntiles=NT):
    with ExitStack() as s:
        bufs = Ring([k.sb(s, "cpb%d" % i, [128, 1024], F32) for i in range(2)])
        for ti in range(ntiles):
            i, b, deps = bufs.get()
            d = k.dma("sp", b[:], src[ti * 128:(ti + 1) * 128, :], deps=deps)
            d2 = k.dma("sp", dst[ti * 128:(ti + 1) * 128, :], b[:], deps=[d])
            bufs.release(i, [d2])
        k.P.barrier()


def phase_l1_prep(k, I, xres, S1):
    nc, P = k.nc, k.P
    m = k.mod[1]
    QD_d, KD_d, KK_d, V_d, GO_d, EL = S1["QD"], S1["KD"], S1["KK"], S1["V"], S1["GO"], S1["EL"]
    with ExitStack() as s:
        win = k.sb(s, "p1_win", [128, 8, 3104], BF16)
        wz = k.sb(s, "p1_wz", [33, 1024], F32)
        tri = k.sb(s, "p1_tri", [128, 4, 128], F32)
        lnS = k.sb(s, "p1_lnS", [128, 1], F32)
        R = mk_norm_rings(k, s, "p1_")
        xt = Ring([k.sb(s, "p1_xt%d" % i, [128, 1024], F32) for i in range(4)])
        hTb = Ring([k.sb(s, "p1_hT%d" % i, [128, 8, 512], BF16) for i in range(2)])
        qk32 = k.sb(s, "p1_qk32", [128, 8, 512], F32)
        gdx = k.sb(s, "p1_gdx", [33, 512], F32)
        qdb = Ring([k.sb(s, "p1_qdb%d" % i, [128, 8, 512], BF16) for i in range(2)])
        kdb = Ring([k.sb(s, "p1_kdb%d" % i, [128, 8, 512], BF16) for i in range(2)])
        spb = k.sb(s, "p1_sp", [128, 1024], F32)
        eD = k.sb(s, "p1_eD", [128, 1024], F32)
        ec = Ring([k.sb(s, "p1_ec%d" % i, [128, 2, 128], F32) for i in range(2)])
        tmo = Ring([k.sb(s, "p1_tmo%d" % i, [128, 3, 1024], BF16) for i in range(2)])
        banks = Ring([k.ps(s, "p1_b%d" % i, [128, 512]) for i in range(4)])
        w_in = I["l1_w_in"]
        for kk in range(8):
            k.dma("pool", win[:, kk, :], w_in[kk * 128:(kk + 1) * 128, :])
        k.dma("sp", wz[:], I["l1_wz"])
        k.dma("sp", tri[:], I["c_tri"])
        k.dma("sp", lnS[:], I["c_lnS"])
        k.memset("dve", gdx[32:33, :], 1.0)
        P.barrier()
        for bi, (s0, n) in enumerate(BLOCKS):
            j = 1 if bi == 0 else 0
            ntl = n // 128
            xs, xrel = [], []
            for t in range(ntl):
                xi, xb, xdeps = xt.get()
                d = k.dma("sp", xb[:], xres[s0 + t * 128:s0 + (t + 1) * 128, :], deps=xdeps)
                xs.append((xb[:], [d])); xrel.append(xi)
            hi, hb, hdeps = hTb.get()
            R["pT"].free[0] = R["pT"].free[0] + hdeps
            tev, _ = k.norm_group(R, xs, m["A1"], m["modT"], j, hb, 0)
            for xi in xrel:
                xt.release(xi, tev)

            def fmproj(c0, M, extra):
                b_i, bank, bdeps = banks.get()
                t = None
                for kk in range(8):
                    t = k.mm(bank[0:M, 0:n], win[:, kk, c0:c0 + M], hb[:, kk, 0:n], kk == 0, kk == 7,
                             deps=(extra + bdeps) if kk == 0 else ())
                return b_i, bank, t
            tqk = []
            for c in range(8):
                b_i, bank, t = fmproj(c * 128, 128, tev if c == 0 else [])
                te = k.cp("act" if c % 2 else "dve", qk32[:, c, 0:n], bank[:, 0:n], deps=[t])
                banks.release(b_i, [te]); tqk.append(te)
            b_i, bank, t = fmproj(3072, 32, [])
            tgd = k.cp("dve", gdx[0:32, 0:n], bank[0:32, 0:n], deps=[t])
            banks.release(b_i, [tgd])
            q_i, qb, qdeps = qdb.get()
            k_i, kb, kdeps = kdb.get()
            lastq = lastk = None
            lastpe = None
            for t in range(ntl):
                ti = s0 // 128 + t
                tc = slice(t * 128, (t + 1) * 128)
                tsp = []
                for half in range(2):
                    b_i, bank, bdeps = banks.get()
                    tz = k.mm(bank[:], gdx[0:33, tc], wz[0:33, half * 512:(half + 1) * 512], True, True, deps=[tgd] + bdeps)
                    t1 = k.act(spb[:, half * 512:(half + 1) * 512], bank[:], AF.Exp, deps=[tz], scale=-1.0)
                    banks.release(b_i, [t1])
                    t2 = k.act(spb[:, half * 512:(half + 1) * 512], spb[:, half * 512:(half + 1) * 512], AF.Ln, deps=[t1], bias=1.0)
                    tsp.append(t2)
                tD = []
                for dr in range(2):
                    b_i, bank, bdeps = banks.get()
                    tm = k.mm(bank[:], tri[:, 2 + dr, :], spb[:, dr * 512:(dr + 1) * 512], True, True, deps=tsp + bdeps)
                    te = k.act(eD[:, dr * 512:(dr + 1) * 512], bank[:], AF.Exp, deps=[tm])
                    banks.release(b_i, [te]); tD.append(te)
                o_i, ob, odeps = tmo.get()
                b_i, bank, bdeps = banks.get()
                tk_ = None
                for kk in range(8):
                    tk_ = k.mm(bank[:], hb[:, kk, tc], win[:, kk, 512:1024], kk == 0, kk == 7, deps=bdeps if kk == 0 else ())
                w1 = k.tt("dve", ob[:, 0, 0:512], bank[:], eD[:, 0:512], ALU.mult, deps=[tk_, tD[0]] + odeps)
                w2 = k.tt("dve", ob[:, 0, 512:1024], bank[:], eD[:, 512:1024], ALU.mult, deps=[tD[1]])
                banks.release(b_i, [w2])
                tl_ = []
                for q4 in range(4):
                    b_i, bank, bdeps = banks.get()
                    c0 = 1024 + q4 * 512
                    tm = None
                    for kk in range(8):
                        tm = k.mm(bank[:], hb[:, kk, tc], win[:, kk, c0:c0 + 512], kk == 0, kk == 7, deps=bdeps if kk == 0 else ())
                    if q4 < 2:
                        te = k.cp("dve", ob[:, 1, q4 * 512:(q4 + 1) * 512], bank[:], deps=[tm])
                    else:
                        te = k.act(ob[:, 2, (q4 - 2) * 512:(q4 - 1) * 512], bank[:], AF.Silu, deps=[tm])
                    banks.release(b_i, [te]); tl_.append(te)
                    lastpe = tm
                d1 = k.dma("pool", KK_d[ti * 128:(ti + 1) * 128, :, :], ob[:, 0, :].rearrange("p (a d) -> p a d", a=2), deps=[w1, w2])
                d2 = k.dma("pool", V_d[ti * 128:(ti + 1) * 128, :], ob[:, 1, :], deps=tl_[0:2])
                d3 = k.dma("pool", GO_d[ti * 128:(ti + 1) * 128, :], ob[:, 2, :], deps=tl_[2:4])
                tmo.release(o_i, [d1, d2, d3])
                for dr in range(2):
                    for h in range(4):
                        b_i, bank, bdeps = banks.get()
                        tm = k.mm(bank[:, 0:128], spb[:, dr * 512 + h * 128:dr * 512 + (h + 1) * 128], tri[:, dr, :], True, True,
                                  deps=tsp + bdeps)
                        e_i, eb, edeps = ec.get()
                        a1 = k.act(eb[:, 0, :], bank[:, 0:128], AF.Exp, deps=[tm] + edeps, bias=lnS[:], scale=1.0)
                        a2 = k.act(eb[:, 1, :], bank[:, 0:128], AF.Exp, deps=[tm], scale=-1.0)
                        lc = 63 if dr == 0 else 0
                        a3 = k.act(EL[:, dr * 4 + h, 2 * ti:2 * ti + 2], bank[:, lc:128:64], AF.Exp, deps=[tm])
                        banks.release(b_i, [a3])
                        lastq = k.tt("dve", qb[:, dr * 4 + h, tc], qk32[:, h, tc], eb[:, 0, :], ALU.mult,
                                     deps=[a1, tqk[h]] + (qdeps if (t == 0 and dr == 0 and h == 0) else []))
                        lastk = k.tt("dve", kb[:, dr * 4 + h, tc], qk32[:, 4 + h, tc], eb[:, 1, :], ALU.mult,
                                     deps=[a2, tqk[4 + h]] + (kdeps if (t == 0 and dr == 0 and h == 0) else []))
                        ec.release(e_i, [lastk])
            dq = k.dma("pool", QD_d[:, :, s0:s0 + n].rearrange("c p t -> p c t"), qb[:, :, 0:n], deps=[lastq])
            dk = k.dma("pool", KD_d[:, :, s0:s0 + n].rearrange("c p t -> p c t"), kb[:, :, 0:n], deps=[lastk])
            qdb.release(q_i, [dq]); kdb.release(k_i, [dk])
            hTb.release(hi, [lastpe])
        P.barrier()


def phase_l1_scan(k, I, S1):
    nc, P = k.nc, k.P
    QD_d, KD_d, KK_d, V_d, O_d, EL = S1["QD"], S1["KD"], S1["KK"], S1["V"], S1["O"], S1["EL"]
    with ExitStack() as s:
        St = k.sb(s, "sc_S", [128, 8, 256], F32)
        Sb = k.sb(s, "sc_Sb", [128, 8, 256], BF16)
        msk = k.sb(s, "sc_msk", [64, 2, 256], BF16)
        inr = Ring([dict(qd=k.sb(s, "sc_qd%d" % i, [128, 4, 64], BF16), kd=k.sb(s, "sc_kd%d" % i, [128, 4, 64], BF16),
                         kk=k.sb(s, "sc_kk%d" % i, [64, 512], BF16), v=k.sb(s, "sc_v%d" % i, [64, 1024], BF16)) for i in range(4)])
        scm = Ring([k.sb(s, "sc_scm%d" % i, [64, 256], BF16) for i in range(2)])
        osb = Ring([k.sb(s, "sc_o%d" % i, [64, 1024], F32) for i in range(3)])
        scb = Ring([k.ps(s, "sc_ps%d" % i, [128, 512]) for i in range(2)])
        obk = Ring([[k.ps(s, "sc_po%d_%d" % (i, c), [128, 512]) for c in range(2)] for i in range(2)])
        kvb = Ring([k.ps(s, "sc_pk%d" % i, [128, 512]) for i in range(2)])
        k.dma("sp", msk[:], I["c_mask"])
        k.memset("dve", St[:], 0.0)
        k.memset("pool", Sb[:], 0.0)
        P.barrier()
        stok = [[None] * 4 for _ in range(2)]
        sdve = [[None] * 4 for _ in range(2)]
        for step in range(68):
            for dr in range(2):
                c = step if dr == 0 else ((3 - step) if step < 4 else (71 - step))
                lat = c >= 4
                i_i, ib, ideps = inr.get()
                cs = slice(c * 64, (c + 1) * 64)
                dl = [k.dma("sp", ib["kk"][:], KK_d[cs, dr, :], deps=ideps),
                      k.dma("sp", ib["v"][:], V_d[cs, :], deps=ideps)]
                if lat:
                    dl.append(k.dma("sp", ib["qd"][:], QD_d[dr * 4:(dr + 1) * 4, :, cs].rearrange("h p t -> p h t"), deps=ideps))
                    dl.append(k.dma("sp", ib["kd"][:], KD_d[dr * 4:(dr + 1) * 4, :, cs].rearrange("h p t -> p h t"), deps=ideps))
                lastpe = None
                if lat:
                    s_i, sbk, sdeps = scb.get()
                    t = None
                    for h in range(4):
                        t = k.mm(sbk[0:64, h * 64:(h + 1) * 64], ib["kd"][:, h, :], ib["qd"][:, h, :], True, True,
                                 deps=(dl + sdeps) if h == 0 else (), sig=(h == 3))
                    m_i, mb, mdeps = scm.get()
                    tmk = k.tt("dve", mb[:], sbk[0:64, 0:256], msk[:, dr, :], ALU.mult, deps=[t] + mdeps)
                    scb.release(s_i, [tmk])
                    o_i, ob, odeps = obk.get()
                    for h in range(4):
                        oo = ob[h // 2][0:64, (h % 2) * 256:(h % 2) * 256 + 256]
                        k.mm(oo, ib["qd"][:, h, :], Sb[:, dr * 4 + h, :], True, False,
                             deps=([stok[dr][h]] if stok[dr][h] else []) + (odeps if h == 0 else []), sig=False)
                        lastpe = k.mm(oo, mb[:, h * 64:(h + 1) * 64], ib["v"][0:64, h * 256:(h + 1) * 256], False, True,
                                      deps=[tmk] if h == 0 else ())
                    scm.release(m_i, [lastpe])
                    os_i, osbuf, osdeps = osb.get()
                    e0 = k.cp("act", osbuf[:, 0:512], ob[0][0:64, :], deps=[lastpe] + osdeps)
                    e1 = k.cp("dve", osbuf[:, 512:1024], ob[1][0:64, :], deps=[lastpe] + osdeps)
                    obk.release(o_i, [e0, e1])
                    dd = k.dma("pool", O_d[dr, (c - 4) * 64:(c - 3) * 64, :], osbuf[:], deps=[e0, e1])
                    osb.release(os_i, [dd])
                for hp in range(2):
                    kv_i, kvbank, kvdeps = kvb.get()
                    t = None
                    for hh in range(2):
                        h = hp * 2 + hh
                        t = k.mm(kvbank[:, hh * 256:(hh + 1) * 256], ib["kk"][0:64, h * 128:(h + 1) * 128],
                                 ib["v"][0:64, h * 256:(h + 1) * 256], True, True,
                                 deps=(dl + kvdeps) if hh == 0 else (), sig=(hh == 1))
                    lastpe = t
                    ups = []
                    for hh in range(2):
                        h = hp * 2 + hh
                        u = k.stt("dve", St[:, dr * 4 + h, :], St[:, dr * 4 + h, :], EL[:, dr * 4 + h, c:c + 1],
                                  kvbank[:, hh * 256:(hh + 1) * 256], ALU.mult, ALU.add,
                                  deps=[t] + ([sdve[dr][h]] if sdve[dr][h] else []))
                        sdve[dr][h] = u
                        ups.append(u)
                        stok[dr][h] = k.cp("act", Sb[:, dr * 4 + h, :], St[:, dr * 4 + h, :], deps=[u, lastpe])
                    kvb.release(kv_i, ups)
                inr.release(i_i, [lastpe])
        P.barrier()


def l1_get_mix_factory(k, I, s, S1):
    O_d, GO_d = S1["O"], S1["GO"]
    gng = k.sb(s, "g_gng", [128, 4, 256], F32)
    for h in range(4):
        k.dma("sp", gng[:, h, :], I["l1_gnorm_g"].to_broadcast([128, 256]))
    oa = Ring([k.sb(s, "g_oa%d" % i, [128, 1024], F32) for i in range(2)])
    ob_ = Ring([k.sb(s, "g_ob%d" % i, [128, 1024], F32) for i in range(2)])
    go = Ring([k.sb(s, "g_go%d" % i, [128, 1024], BF16) for i in range(2)])
    o2 = Ring([k.sb(s, "g_o2%d" % i, [128, 1024], BF16) for i in range(2)])
    st = Ring([k.sb(s, "g_st%d" % i, [128, 12], F32) for i in range(2)])
    junk = k.sb(s, "g_junk", [128, 256], BF16)
    mixr = Ring([k.sb(s, "g_mix%d" % i, [128, 8, 512], BF16) for i in range(2)])

    def get_mix(bi, s0, n):
        R = k.R_cur
        m_i, mb, mdeps = mixr.get()
        p_i, pT, pdeps = R["pT"].get()
        ttr = []
        for t in range(n // 128):
            r0 = s0 - TC + t * 128
            a_i, ab, adeps = oa.get()
            b_i, bb, bdeps = ob_.get()
            g_i, gb, gdeps = go.get()
            d1 = k.dma("sp", ab[:], O_d[0, r0:r0 + 128, :], deps=adeps)
            d2 = k.dma("sp", bb[:], O_d[1, r0:r0 + 128, :], deps=bdeps)
            d3 = k.dma("sp", gb[:], GO_d[s0 + t * 128:s0 + (t + 1) * 128, :], deps=gdeps)
            t0 = k.tt("pool", ab[:], ab[:], bb[:], ALU.add, deps=[d1, d2])
            ob_.release(b_i, [t0])
            s_i, sb_, sdeps = st.get()
            tq = []
            for h in range(4):
                tq.append(k.act(junk[:], ab[:, h * 256:(h + 1) * 256], AF.Square, deps=[t0] + sdeps, accum_out=sb_[:, h:h + 1]))
            t1 = k.act(sb_[:, 4:8], sb_[:, 0:4], AF.Sqrt, deps=tq, scale=1.0 / 256, bias=k.eps[:])
            t2 = k.recip(sb_[:, 8:12], sb_[:, 4:8], deps=[t1])
            t3 = None
            for h in range(4):
                t3 = k.stt("dve", ab[:, h * 256:(h + 1) * 256], ab[:, h * 256:(h + 1) * 256], sb_[:, 8 + h:9 + h], gng[:, h, :],
                           ALU.mult, ALU.mult, deps=[t2])
            st.release(s_i, [t3])
            o_i, o2b, o2deps = o2.get()
            t4 = k.tt("pool", o2b[:], ab[:], gb[:], ALU.mult, deps=[t3, d3] + o2deps)
            oa.release(a_i, [t4]); go.release(g_i, [t4])
            last = None
            for c in range(8):
                last = k.tr(pT[:, c, t * 128:(t + 1) * 128], o2b[:, c * 128:(c + 1) * 128], k.identb[:],
                            deps=([t4] + pdeps) if c == 0 else (), sig=(c == 7))
            o2.release(o_i, [last])
            ttr.append(last)
        te = k.cp("dve", mb[:, :, 0:n], pT[:, :, 0:n], deps=ttr + mdeps)
        R["pT"].release(p_i, [te])
        return mb, [te], (lambda toks, i=m_i: mixr.release(i, toks))
    return get_mix


def kernel(**inputs):
    nc = build()
    maps = [host_inputs(inputs, b) for b in range(8)]
    res = run_bass_kernel_spmd(nc, maps, core_ids=list(range(8)))
    return np.stack([np.asarray(res.results[b]["out"], np.float32) for b in range(8)], 0)
```

```python
import numpy as np
import ml_dtypes
from contextlib import ExitStack
import concourse.bass as bass
import concourse.mybir as mybir
from concourse.bass_utils import run_bass_kernel_spmd

F32 = mybir.dt.float32
BF16 = mybir.dt.bfloat16
AF = mybir.ActivationFunctionType
ALU = mybir.AluOpType
AX = mybir.AxisListType

T = 4352
NT = 34
TC = 256
TL = 4096
D = 1024
BLOCKS = [(0, 256)] + [(256 + 512 * i, 512) for i in range(8)]
EPS = 1e-6
NE = 32
ATT_SCALE = 192.0 ** -0.5

STOP_AFTER = None
SUB = 99
L1_ONLY = False
DEBUG_OUT = False

ENGS = ("pe", "act", "dve", "pool", "sp")
N_DMA_SEMS_HW = 48
N_DMA_SEMS_SW = 32
N_DMA_SEMS = N_DMA_SEMS_HW + N_DMA_SEMS_SW


class Prog:
    def __init__(self, nc, es):
        self.nc = nc
        self.streams = {e: [] for e in ENGS}
        self.count = {e: 0 for e in ENGS}
        self.esem = {e: es.enter_context(nc.semaphore("s_" + e)) for e in ENGS if e != "sp"}
        self.dsem = [es.enter_context(nc.semaphore("d%d" % i)) for i in range(N_DMA_SEMS)]
        self.dval = [0] * N_DMA_SEMS
        self.dnext = 0
        self.dnext_sw = 0
        self.waited = {e: {} for e in ENGS}
        self.n_inst = 0

    def _wait(self, eng, tok):
        if tok is None:
            return
        key = (tok[0], tok[1])
        n = tok[2]
        if self.waited[eng].get(key, 0) >= n:
            return
        self.waited[eng][key] = n
        self.streams[eng].append(("wait", key, n))

    def op(self, eng, fn, deps=(), sig=True):
        for d in deps:
            self._wait(eng, d)
        self.n_inst += 1
        if sig:
            self.count[eng] += 1
            self.streams[eng].append(("op", fn, True))
            return ("e", eng, self.count[eng])
        self.streams[eng].append(("op", fn, False))
        return None

    def dma(self, eng, out, in_, deps=(), **kw):
        for d in deps:
            self._wait(eng, d)
        if eng == "pool":
            idx = N_DMA_SEMS_HW + self.dnext_sw
            self.dnext_sw = (self.dnext_sw + 1) % N_DMA_SEMS_SW
        else:
            idx = self.dnext
            self.dnext = (self.dnext + 1) % N_DMA_SEMS_HW
        if self.dval[idx] > 0:
            self._wait(eng, ("d", idx, self.dval[idx]))
        self.dval[idx] += 16
        self.n_inst += 1
        self.streams[eng].append(("dma", out, in_, idx, kw))
        return ("d", idx, self.dval[idx])

    def barrier(self):
        toks = [("e", e, self.count[e]) for e in ENGS if e != "sp" and self.count[e] > 0]
        toks += [("d", i, v) for i, v in enumerate(self.dval) if v > 0]
        for e in ENGS:
            for t in toks:
                self._wait(e, t)

    def emit(self):
        nc = self.nc
        with nc.Block() as block:
            def run(engname, e):
                for item in self.streams[engname]:
                    if item[0] == "wait":
                        _, key, n = item
                        sem = self.esem[key[1]] if key[0] == "e" else self.dsem[key[1]]
                        e.wait_ge(sem, n)
                    elif item[0] == "op":
                        ins = item[1](e)
                        if item[2]:
                            ins.then_inc(self.esem[engname], 1)
                    else:
                        _, out, in_, idx, kw = item
                        e.dma_start(out=out, in_=in_, **kw).then_inc(self.dsem[idx], 16)

            @block.tensor
            def _(e):
                run("pe", e)

            @block.scalar
            def _(e):
                run("act", e)

            @block.vector
            def _(e):
                run("dve", e)

            @block.gpsimd
            def _(e):
                run("pool", e)

            @block.sync
            def _(e):
                run("sp", e)


class Ring:
    def __init__(self, bufs):
        self.bufs = list(bufs)
        self.free = [[] for _ in self.bufs]
        self.i = 0

    def get(self):
        i = self.i
        self.i = (i + 1) % len(self.bufs)
        deps = self.free[i]
        self.free[i] = []
        return i, self.bufs[i], list(deps)

    def release(self, i, toks):
        self.free[i] = [t for t in toks if t is not None]


class K:
    def __init__(self, nc, es):
        self.nc = nc
        self.P = Prog(nc, es)
        self.es = es

    def mm(self, out, lhsT, rhs, start, stop, deps=(), sig=None):
        if sig is None:
            sig = stop
        return self.P.op("pe", lambda e: e.matmul(out, lhsT=lhsT, rhs=rhs, start=start, stop=stop), deps, sig)

    def tr(self, out, in_, ident, deps=(), sig=True):
        return self.P.op("pe", lambda e: e.transpose(out, in_, ident), deps, sig)

    def act(self, out, in_, func, deps=(), **kw):
        return self.P.op("act", lambda e: e.activation(out=out, in_=in_, func=func, **kw), deps)

    def ts(self, eng, out, in0, s1, s2, op0, op1=None, deps=()):
        if op1 is None:
            return self.P.op(eng, lambda e: e.tensor_scalar(out=out, in0=in0, scalar1=s1, scalar2=None, op0=op0), deps)
        return self.P.op(eng, lambda e: e.tensor_scalar(out=out, in0=in0, scalar1=s1, scalar2=s2, op0=op0, op1=op1), deps)

    def stt(self, eng, out, in0, scalar, in1, op0, op1, deps=()):
        return self.P.op(eng, lambda e: e.scalar_tensor_tensor(out=out, in0=in0, scalar=scalar, in1=in1, op0=op0, op1=op1), deps)

    def tt(self, eng, out, in0, in1, op, deps=()):
        return self.P.op(eng, lambda e: e.tensor_tensor(out=out, in0=in0, in1=in1, op=op), deps)

    def cp(self, eng, out, in_, deps=()):
        if eng == "act":
            return self.P.op("act", lambda e: e.activation(out=out, in_=in_, func=AF.Copy), deps)
        return self.P.op(eng, lambda e: e.tensor_copy(out=out, in_=in_), deps)

    def recip(self, out, in_, deps=()):
        return self.P.op("dve", lambda e: e.reciprocal(out=out, in_=in_), deps)

    def memset(self, eng, ap, val, deps=()):
        return self.P.op(eng, lambda e: e.memset(ap, val), deps)

    def dma(self, eng, out, in_, deps=()):
        return self.P.dma(eng, out, in_, deps)

    def _uniq(self, name):
        self._nctr = getattr(self, "_nctr", 0) + 1
        return "%s_%d" % (name, self._nctr)

    def sb(self, es, name, shape, dt):
        return es.enter_context(self.nc.sbuf_tensor(self._uniq(name), shape, dt))

    def ps(self, es, name, shape, dt=F32):
        return es.enter_context(self.nc.psum_tensor(self._uniq(name), shape, dt))

    def norm_group(self, R, xs, A, SH, j, dst, c0):
        n = len(xs)
        si, ssq, sdeps = R["ssq"].get()
        tsq = []
        for t, (xt, deps) in enumerate(xs):
            jt = R.get("junk_tok")
            tq_ = self.act(R["junk"][:], xt, AF.Square, deps=list(deps) + sdeps + ([jt] if jt else []), accum_out=ssq[:, t:t + 1])
            R["junk_tok"] = tq_
            tsq.append(tq_)
        tq = self.act(ssq[:, 4:4 + n], ssq[:, 0:n], AF.Sqrt, deps=tsq, scale=1.0 / D, bias=self.eps[:])
        trc = self.recip(ssq[:, 8:8 + n], ssq[:, 4:4 + n], deps=[tq])
        pi, pT, pdeps = R["pT"].get()
        ttr = []
        xn_rel = []
        for t, (xt, deps) in enumerate(xs):
            xi, xnb, xdeps = R["xnb"].get()
            tx = self.act(xnb[:], xt, AF.Copy, deps=[trc] + xdeps, scale=ssq[:, 8 + t:9 + t])
            last = None
            for k in range(8):
                last = self.tr(pT[:, k, t * 128:(t + 1) * 128], xnb[:, k * 128:(k + 1) * 128], self.identb[:],
                               deps=([tx] + pdeps) if k == 0 else (), sig=(k == 7))
            R["xnb"].release(xi, [last])
            ttr.append(last)
        R["ssq"].release(si, [tx])
        tev = []
        for k in range(8):
            o = dst[:, k, c0:c0 + n * 128]
            i_ = pT[:, k, 0:n * 128]
            tev.append(self.ts("dve", o, i_, A[:, k, j:j + 1], SH[:, k, j:j + 1], ALU.mult, ALU.add, deps=ttr))
        R["pT"].release(pi, tev[-2:])
        return tev[-2:], tsq

    def gating(self, R, lg_ps, tile, Gall, rb, deps):
        i, g, gdeps = R["gate"].get()
        lgs = g[:, 0:32]
        m8 = g[:, 32:40]
        negm = g[:, 40:41]
        mask = g[:, 48:80]
        ex = g[:, 80:112]
        ssum = g[:, 112:113]
        t1 = self.tt("dve", lgs, lg_ps, rb[:], ALU.add, deps=list(deps) + gdeps)
        t2 = self.P.op("dve", lambda e: e.max(out=m8, in_=lgs), [t1])
        t3 = self.ts("dve", mask, lgs, m8[:, 3:4], None, ALU.is_ge, deps=[t2])
        t4 = self.ts("dve", negm, m8[:, 0:1], -1.0, None, ALU.mult, deps=[t3])
        t5 = self.act(ex, lgs, AF.Exp, deps=[t4], bias=negm, scale=1.0)
        t6 = self.tt("dve", ex, ex, mask, ALU.mult, deps=[t5])
        t7 = self.P.op("dve", lambda e: e.reduce_sum(out=ssum, in_=ex, axis=AX.X), [t6])
        t8 = self.recip(ssum, ssum, deps=[t7])
        t9 = self.ts("dve", Gall[:, tile, :], ex, ssum, None, ALU.mult, deps=[t8])
        R["gate"].release(i, [t9])
        return t1, t9


def fm(v):
    v = np.asarray(v)
    return np.ascontiguousarray(v.reshape(-1, 128).T)


def phase_setup(k, I):
    nc, P = k.nc, k.P
    es = k.es
    k.identb = k.sb(es, "identb", [128, 128], BF16)
    k.identf = k.sb(es, "identf", [128, 128], F32)
    k.onesb = k.sb(es, "onesb", [128, 128], BF16)
    k.eps = k.sb(es, "eps", [128, 1], F32)
    k.scfm = k.sb(es, "scfm", [128, 8, 2], F32)
    k.Gall = k.sb(es, "Gall", [128, NT, NE], F32)
    k.dma("sp", k.identb[:], I["c_identb"])
    k.dma("sp", k.identf[:], I["c_identf"])
    k.dma("sp", k.scfm[:], I["cfm"])
    k.memset("dve", k.onesb[:], 1.0)
    k.memset("dve", k.eps[:], EPS)
    P.barrier()
    k.act(k.scfm[:], k.scfm[:], AF.Silu)
    P.barrier()


def phase_adaln(k, I, l):
    nc, P = k.nc, k.P
    es = k.es
    modT = k.sb(es, "modT%d" % l, [128, 48, 2], F32)
    A1 = k.sb(es, "A1_%d" % l, [128, 8, 2], F32)
    A2 = k.sb(es, "A2_%d" % l, [128, 8, 2], F32)
    gd = nc.dram_tensor("gd%d" % l, [2, 2048], F32, kind="Internal").ap()
    modw = I["l%d_mod_w" % l]
    with ExitStack() as s:
        wp = Ring([k.sb(s, "ad_wp%d" % i, [128, 8, 1024], F32) for i in range(2)])
        psfm = k.ps(s, "ad_psfm", [128, 48, 2])
        pstm = k.ps(s, "ad_pstm", [2, 2048])
        brow = k.sb(s, "ad_brow", [2, 2048], F32)
        gts = k.sb(s, "ad_gts", [2, 2048], F32)
        mb = k.sb(s, "ad_mb", [128, 48], F32)
        ng = k.sb(s, "ad_ng", [128, 16], F32)
        tmp = k.sb(s, "ad_tmp", [128, 16, 2], F32)
        d0 = [k.dma("sp", mb[:], I["l%d_mod_b_fm" % l]),
              k.dma("sp", ng[:, 0:8], I["l%d_norm1_g_fm" % l]),
              k.dma("sp", ng[:, 8:16], I["l%d_norm2_g_fm" % l])]
        mbrow = I["l%d_mod_b" % l]
        d0.append(k.dma("sp", brow[:, 0:1024], mbrow[:, 2048:3072].to_broadcast([2, 1024])))
        d0.append(k.dma("sp", brow[:, 1024:2048], mbrow[:, 5120:6144].to_broadcast([2, 1024])))
        tl = None
        for pc in range(6):
            i, buf, wdeps = wp.get()
            dts = [k.dma("sp", buf[:, kk, :], modw[kk * 128:(kk + 1) * 128, pc * 1024:(pc + 1) * 1024], deps=wdeps)
                   for kk in range(8)]
            t = None
            for jj in range(8):
                j = pc * 8 + jj
                for kk in range(8):
                    t = k.mm(psfm[:, j, :], buf[:, kk, jj * 128:(jj + 1) * 128], k.scfm[:, kk, :], kk == 0, kk == 7,
                             deps=dts if (jj == 0 and kk == 0) else ())
            if pc in (2, 5):
                gi = 0 if pc == 2 else 1
                for half in range(2):
                    for kk in range(8):
                        t = k.mm(pstm[:, gi * 1024 + half * 512:gi * 1024 + (half + 1) * 512], k.scfm[:, kk, :],
                                 buf[:, kk, half * 512:(half + 1) * 512], kk == 0, kk == 7)
            wp.release(i, [t])
            tl = t
        e1 = k.tt("dve", modT[:, :, 0], psfm[:, :, 0], mb[:], ALU.add, deps=[tl] + d0)
        e2 = k.tt("dve", modT[:, :, 1], psfm[:, :, 1], mb[:], ALU.add, deps=[tl])
        e3 = k.tt("dve", gts[:], pstm[:], brow[:], ALU.add, deps=[tl])
        dg = k.dma("sp", gd, gts[:], deps=[e3])
        e4 = k.ts("dve", tmp[:, 0:8, :], modT[:, 8:16, :], 1.0, None, ALU.add, deps=[e1, e2])
        e5 = k.ts("dve", tmp[:, 8:16, :], modT[:, 32:40, :], 1.0, None, ALU.add, deps=[e4])
        for j in range(2):
            k.tt("dve", A1[:, :, j], tmp[:, 0:8, j], ng[:, 0:8], ALU.mult, deps=[e5])
            k.tt("dve", A2[:, :, j], tmp[:, 8:16, j], ng[:, 8:16], ALU.mult, deps=[e5])
        P.barrier()
    k.mod[l] = dict(modT=modT, A1=A1, A2=A2, gd=gd)


def load_gate_rows(k, s, l, which, name):
    gb = k.sb(s, name, [128, 2, 1024], F32)
    gd = k.mod[l]["gd"]
    for j in range(2):
        k.dma("sp", gb[:, j, :], gd[j:j + 1, which * 1024:(which + 1) * 1024].to_broadcast([128, 1024]))
    return gb


def mk_norm_rings(k, s, pfx):
    R = {}
    R["ssq"] = Ring([k.sb(s, pfx + "ssq%d" % i, [128, 12], F32) for i in range(2)])
    R["junk"] = k.sb(s, pfx + "junk", [128, 1024], BF16)
    R["xnb"] = Ring([k.sb(s, pfx + "xnb%d" % i, [128, 1024], BF16) for i in range(4)])
    R["pT"] = Ring([k.ps(s, pfx + "pT%d" % i, [128, 8, 512], BF16) for i in range(1)])
    return R


def phase_l0_inproj(k, I, cqn, ckvn, krT, UT, cosT, sinT):
    nc, P = k.nc, k.P
    m = k.mod[0]
    with ExitStack() as s:
        win = k.sb(s, "win", [128, 8, 1024], BF16)
        gfm = k.sb(s, "gfm", [128, 5], F32)
        R = mk_norm_rings(k, s, "ip_")
        xt = Ring([k.sb(s, "ip_xt%d" % i, [128, 1024], F32) for i in range(4)])
        hTb = Ring([k.sb(s, "ip_hT%d" % i, [128, 8, 512], BF16) for i in range(2)])
        sq = k.sb(s, "ip_sq", [128, 3, 512], BF16)
        r32 = k.sb(s, "ip_r32", [128, 2, 512], F32)
        t12 = k.sb(s, "ip_t12", [64, 2, 512], F32)
        banks = Ring([k.ps(s, "ip_b%d" % i, [128, 512]) for i in range(4)])
        w_in = I["l0_w_in"]
        for kk in range(8):
            k.dma("pool", win[:, kk, 0:704], w_in[kk * 128:(kk + 1) * 128, 0:704])
            k.dma("pool", win[:, kk, 768:1024], w_in[kk * 128:(kk + 1) * 128, 704:960])
        k.dma("sp", gfm[:, 0:3], I["l0_q_norm_g_fm"])
        k.dma("sp", gfm[:, 3:5], I["l0_kv_norm_g_fm"])
        P.barrier()
        k.ts("dve", win[:, :, 704:720], win[:, :, 656:672], -1.0, None, ALU.mult)
        k.cp("dve", win[:, :, 720:736], win[:, :, 640:656])
        k.ts("dve", win[:, :, 736:752], win[:, :, 688:704], -1.0, None, ALU.mult)
        k.cp("dve", win[:, :, 752:768], win[:, :, 672:688])
        P.barrier()
        xin = I["xin"]
        t12_last = []
        r32_last = [[], []]
        if SUB == 1:
            return
        for bi, (s0, n) in enumerate(BLOCKS):
            if SUB < 99 and bi > 0:
                break
            j = 1 if bi == 0 else 0
            xs = []
            xrel = []
            for t in range(n // 128):
                xi, xb, xdeps = xt.get()
                d = k.dma("sp", xb[:], xin[s0 + t * 128:s0 + (t + 1) * 128, :], deps=xdeps)
                xs.append((xb[:], [d]))
                xrel.append(xi)
            hi, hb, hdeps = hTb.get()
            R["pT"].free[0] = R["pT"].free[0] + hdeps
            tev, tsq = k.norm_group(R, xs, m["A1"], m["modT"], j, hb, 0)
            for xi in xrel:
                xt.release(xi, tev)
            if SUB == 2:
                break

            def proj(c0, M, first_deps):
                bi_, bank, bdeps = banks.get()
                t = None
                for kk in range(8):
                    t = k.mm(bank[0:M, 0:n], win[:, kk, c0:c0 + M], hb[:, kk, 0:n], kk == 0, kk == 7,
                             deps=(first_deps + bdeps) if kk == 0 else ())
                return bi_, bank, t

            def rms_chunks(chunks, dstT, gcol, inv_n, r32s, slot):
                tsqs = []
                for c, (b_i, bank, tk) in enumerate(chunks):
                    tsqs.append(k.act(sq[:, c, 0:n], bank[:, 0:n], AF.Square, deps=[tk]))
                pi, pb, pdeps = banks.get()
                t = None
                for c in range(len(chunks)):
                    t = k.mm(pb[:, 0:n], k.onesb[:], sq[:, c, 0:n], c == 0, c == len(chunks) - 1,
                             deps=(tsqs + pdeps) if c == 0 else ())
                ta = k.act(r32s[:, 0:n], pb[:, 0:n], AF.Sqrt, deps=[t] + r32_last[slot], scale=inv_n, bias=k.eps[:])
                tb = k.recip(r32s[:, 0:n], r32s[:, 0:n], deps=[ta])
                banks.release(pi, [ta])
                outs = []
                for c, (b_i, bank, tk) in enumerate(chunks):
                    to = k.stt("dve", dstT[:, c, s0:s0 + n], bank[:, 0:n], gfm[:, gcol + c:gcol + c + 1], r32s[:, 0:n],
                               ALU.mult, ALU.mult, deps=[tb])
                    banks.release(b_i, [to])
                    outs.append(to)
                r32_last[slot] = [outs[-1]]
                return outs

            qch = [proj(c * 128, 128, tev if c == 0 else []) for c in range(3)]
            rms_chunks(qch, cqn, 0, 1.0 / 384, r32[:, 0, :], 0)
            if SUB == 3:
                break
            kvch = [proj(384 + c * 128, 128, []) for c in range(2)]
            tkv = rms_chunks(kvch, ckvn, 3, 1.0 / 256, r32[:, 1, :], 1)
            ai, abank, ta = proj(640, 64, [])
            bi2, bbank, tb = proj(704, 64, [])
            t1 = k.tt("dve", t12[:, 0, 0:n], abank[0:64, 0:n], cosT[:, s0:s0 + n], ALU.mult, deps=[ta] + t12_last)
            t2 = k.tt("dve", t12[:, 1, 0:n], bbank[0:64, 0:n], sinT[:, s0:s0 + n], ALU.mult, deps=[tb])
            t3 = k.tt("dve", krT[:, s0:s0 + n], t12[:, 0, 0:n], t12[:, 1, 0:n], ALU.add, deps=[t1, t2])
            t12_last = [t3]
            banks.release(ai, [t1])
            banks.release(bi2, [t2])
            tlast = None
            for c in range(2):
                ui, ubank, tu = proj(768 + c * 128, 128, [])
                tc = k.cp("act", UT[:, c, s0:s0 + n], ubank[:, 0:n], deps=[tu])
                banks.release(ui, [tc])
                tlast = tu
            hTb.release(hi, [tlast])
        P.barrier()


def phase_l0_fourier(k, I, UT, mix_d):
    nc, P = k.nc, k.P
    with ExitStack() as s:
        Z = k.sb(s, "f_Z", [128, NT, 512], BF16)
        CS = k.sb(s, "f_CS", [128, 256], BF16)
        d256 = k.sb(s, "f_d256", [128, 2, 2, 256], BF16)
        dbuf = Ring([k.sb(s, "f_db%d" % i, [128, 8, 2, 512], BF16) for i in range(2)])
        fst = Ring([k.sb(s, "f_st%d" % i, [128, 2, 512], BF16) for i in range(2)])
        banks = Ring([k.ps(s, "f_b%d" % i, [128, 512]) for i in range(4)])
        acc = Ring([[k.ps(s, "f_a%d_%d" % (i, c), [128, 512]) for c in range(2)] for i in range(2)])
        k.dma("sp", CS[:], I["c_cs64"])
        for cs_ in range(2):
            k.dma("sp", d256[:, :, cs_, :], I["c_dft256"][:, cs_, :].rearrange("(nt p) k -> p nt k", p=128))
        P.barrier()
        zt = []
        for i in range(NT):
            b_i, bank, bdeps = banks.get()
            t = None
            for c in range(2):
                t = k.mm(bank[:, c * 256:(c + 1) * 256], UT[:, c, i * 128:(i + 1) * 128], CS[:], True, True,
                         deps=bdeps if c == 0 else (), sig=(c == 1))
            te = k.cp("act" if i % 2 else "dve", Z[:, i, :], bank[:], deps=[t])
            banks.release(b_i, [te])
            zt.append(te)
        P.barrier()

        def evac(a_i, ab, tlast, s0, n):
            f_i, fb, fdeps = fst.get()
            t0 = k.cp("act", fb[:, 0, 0:n], ab[0][:, 0:n], deps=[tlast] + fdeps)
            t1 = k.cp("dve", fb[:, 1, 0:n], ab[1][:, 0:n], deps=[tlast] + fdeps)
            acc.release(a_i, [t0, t1])
            d = k.dma("pool", mix_d[6:8, :, s0:s0 + n].rearrange("c p t -> p c t"), fb[:, :, 0:n], deps=[t0, t1])
            fst.release(f_i, [d])

        a_i, ab, adeps = acc.get()
        t = None
        for nt in range(2):
            for c in range(2):
                k.mm(ab[c][:, 0:256], Z[:, nt, c * 256:c * 256 + 128], d256[:, nt, 0, :], nt == 0, False,
                     deps=adeps if (nt == 0 and c == 0) else (), sig=False)
                t = k.mm(ab[c][:, 0:256], Z[:, nt, c * 256 + 128:c * 256 + 256], d256[:, nt, 1, :], False, nt == 1)
        evac(a_i, ab, t, 0, 256)
        dft = I["c_dft4096"]
        for kb in range(8):
            a_i, ab, adeps = acc.get()
            t = None
            for pc in range(4):
                d_i, db, ddeps = dbuf.get()
                dd0 = k.dma("sp", db[:, :, 0, :], dft[pc * 1024:(pc + 1) * 1024, 0, kb * 512:(kb + 1) * 512]
                            .rearrange("(nt p) k -> p nt k", p=128), deps=ddeps)
                dd = k.dma("sp", db[:, :, 1, :], dft[pc * 1024:(pc + 1) * 1024, 1, kb * 512:(kb + 1) * 512]
                           .rearrange("(nt p) k -> p nt k", p=128), deps=ddeps)
                for nt in range(8):
                    zi = 2 + pc * 8 + nt
                    first = (pc == 0 and nt == 0)
                    last = (pc == 3 and nt == 7)
                    for c in range(2):
                        k.mm(ab[c][:], Z[:, zi, c * 256:c * 256 + 128], db[:, nt, 0, :], first, False,
                             deps=([dd0, dd] + (adeps if first else [])) if (nt == 0 and c == 0) else (), sig=False)
                        t = k.mm(ab[c][:], Z[:, zi, c * 256 + 128:c * 256 + 256], db[:, nt, 1, :], False, last,
                                 sig=(c == 1 and (last or nt == 7)))
                dbuf.release(d_i, [t])
            evac(a_i, ab, t, 256 + kb * 512, 512)
        P.barrier()


def phase_l0_attn(k, I, cqn, ckvn, krT, cosT, sinT, mix_d):
    nc, P = k.nc, k.P
    with ExitStack() as s:
        wq = k.sb(s, "a_wq", [128, 3, 6, 256], BF16)
        wkv = k.sb(s, "a_wkv", [128, 2, 6, 256], BF16)
        qn = [k.sb(s, "a_qn%d" % i, [128, T], BF16) for i in range(2)]
        qr = [k.sb(s, "a_qr%d" % i, [64, T], BF16) for i in range(2)]
        kn = [k.sb(s, "a_kn%d" % i, [128, T], BF16) for i in range(2)]
        vv = [k.sb(s, "a_vv%d" % i, [128, NT, 128], BF16) for i in range(2)]
        t12 = k.sb(s, "a_t12", [64, 2, 512], F32)
        pTr = Ring([k.sb(s, "a_pT%d" % i, [128, 512], BF16) for i in range(4)])
        rden = Ring([k.sb(s, "a_rd%d" % i, [128, 512], F32) for i in range(2)])
        ost = Ring([k.sb(s, "a_os%d" % i, [128, 512], BF16) for i in range(2)])
        banks = Ring([k.ps(s, "a_b%d" % i, [128, 512]) for i in range(4)])
        oacc = Ring([k.ps(s, "a_o%d" % i, [128, 512]) for i in range(2)])
        dacc = Ring([k.ps(s, "a_d%d" % i, [128, 512]) for i in range(2)])
        wqu = I["l0_w_q_up"]
        wkvu = I["l0_w_kv_up"]
        for c in range(3):
            k.dma("pool", wq[:, c, :, 0:192], wqu[c * 128:(c + 1) * 128, :].rearrange("p (h j) -> p h j", j=192))
        for c in range(2):
            k.dma("pool", wkv[:, c, :, :], wkvu[c * 128:(c + 1) * 128, :].rearrange("p (h j) -> p h j", j=256))
        P.barrier()
        for c in range(3):
            k.ts("dve", wq[:, c, :, 192:208], wq[:, c, :, 144:160], -1.0, None, ALU.mult)
            k.cp("dve", wq[:, c, :, 208:224], wq[:, c, :, 128:144])
            k.ts("dve", wq[:, c, :, 224:240], wq[:, c, :, 176:192], -1.0, None, ALU.mult)
            k.cp("dve", wq[:, c, :, 240:256], wq[:, c, :, 160:176])
        P.barrier()

        war = [[], []]
        t12_last = []

        def proj(h):
            b = h % 2
            toks = []
            for bi, (s0, n) in enumerate(BLOCKS):
                def grp(lhs_list, rhs_src, M):
                    b_i, bank, bdeps = banks.get()
                    t = None
                    for c, lhsT in enumerate(lhs_list):
                        t = k.mm(bank[0:M, 0:n], lhsT, rhs_src[:, c, s0:s0 + n], c == 0, c == len(lhs_list) - 1,
                                 deps=bdeps if c == 0 else ())
                    return b_i, bank, t
                wdeps = war[b] if bi == 0 else []
                b_i, bank, t = grp([wq[:, c, h, 0:128] for c in range(3)], cqn, 128)
                te = k.cp("act", qn[b][:, s0:s0 + n], bank[:, 0:n], deps=[t] + wdeps)
                banks.release(b_i, [te]); toks.append(te)
                b_i, bank, t = grp([wkv[:, c, h, 0:128] for c in range(2)], ckvn, 128)
                te = k.cp("dve", kn[b][:, s0:s0 + n], bank[:, 0:n], deps=[t] + wdeps)
                banks.release(b_i, [te]); toks.append(te)
                a_i, abank, ta = grp([wq[:, c, h, 128:192] for c in range(3)], cqn, 64)
                b2_i, bbank, tb = grp([wq[:, c, h, 192:256] for c in range(3)], cqn, 64)
                t1 = k.tt("dve", t12[:, 0, 0:n], abank[0:64, 0:n], cosT[:, s0:s0 + n], ALU.mult, deps=[ta] + t12_last)
                t2 = k.tt("dve", t12[:, 1, 0:n], bbank[0:64, 0:n], sinT[:, s0:s0 + n], ALU.mult, deps=[tb])
                t3 = k.tt("dve", qr[b][:, s0:s0 + n], t12[:, 0, 0:n], t12[:, 1, 0:n], ALU.add, deps=[t1, t2] + wdeps)
                t12_last[:] = [t3]
                banks.release(a_i, [t1]); banks.release(b2_i, [t2]); toks.append(t3)
                nt = n // 128
                b_i, bank, bdeps = banks.get()
                t = None
                for tt_ in range(nt):
                    ti = s0 // 128 + tt_
                    for c in range(2):
                        t = k.mm(bank[:, tt_ * 128:(tt_ + 1) * 128], ckvn[:, c, ti * 128:(ti + 1) * 128], wkv[:, c, h, 128:256],
                                 c == 0, c == 1, deps=bdeps if (tt_ == 0 and c == 0) else (), sig=(c == 1 and tt_ == nt - 1))
                te = k.cp("act", vv[b][:, s0 // 128:s0 // 128 + nt, :], bank[:, 0:n].rearrange("p (t d) -> p t d", d=128),
                          deps=[t] + wdeps)
                banks.release(b_i, [te]); toks.append(te)
            return toks

        def attn(h, ptoks):
            b = h % 2
            lastpe = None
            for bi, (s0, n) in enumerate(BLOCKS):
                ktiles = [0, 1] if bi == 0 else list(range(NT))
                o_i, ob, odeps = oacc.get()
                d_i, db, ddeps = dacc.get()
                pend = None
                nk = len(ktiles)
                for idx in range(nk + 1):
                    cur = None
                    if idx < nk:
                        kt = ktiles[idx]
                        s_i, sbk, sdeps = banks.get()
                        k.mm(sbk[:, 0:n], kn[b][:, kt * 128:(kt + 1) * 128], qn[b][:, s0:s0 + n], True, False,
                             deps=sdeps + (ptoks if (bi == 0 and idx == 0) else []), sig=False)
                        t = k.mm(sbk[:, 0:n], krT[:, kt * 128:(kt + 1) * 128], qr[b][:, s0:s0 + n], False, True)
                        p_i, pb, pdeps = pTr.get()
                        te = k.act(pb[:, 0:n], sbk[:, 0:n], AF.Exp, deps=[t] + pdeps, scale=ATT_SCALE)
                        banks.release(s_i, [te])
                        cur = (kt, p_i, pb, te, idx)
                    if pend is not None:
                        kt, p_i, pb, te, ii = pend
                        k.mm(ob[:, 0:n], vv[b][:, kt, :], pb[:, 0:n], ii == 0, ii == nk - 1,
                             deps=[te] + (odeps + ddeps if ii == 0 else []), sig=False)
                        t2 = k.mm(db[:, 0:n], k.onesb[:], pb[:, 0:n], ii == 0, ii == nk - 1, sig=True)
                        pTr.release(p_i, [t2])
                        lastpe = t2
                    pend = cur
                r_i, rb, rdeps = rden.get()
                trr = k.recip(rb[:, 0:n], db[:, 0:n], deps=[lastpe] + rdeps)
                dacc.release(d_i, [trr])
                os_i, osb, osdeps = ost.get()
                to = k.tt("dve", osb[:, 0:n], ob[:, 0:n], rb[:, 0:n], ALU.mult, deps=[trr] + osdeps)
                oacc.release(o_i, [to])
                rden.release(r_i, [to])
                dd = k.dma("sp", mix_d[h, :, s0:s0 + n], osb[:, 0:n], deps=[to])
                ost.release(os_i, [dd])
            war[b] = [lastpe]

        pt = proj(0)
        for h in range(6):
            nxt = proj(h + 1) if h + 1 < 6 else None
            attn(h, pt)
            pt = nxt
        P.barrier()


def phase_wout(k, I, l, get_mix, xsrc, xres, h2T_d, tiles_range):
    nc, P = k.nc, k.P
    m = k.mod[l]
    with ExitStack() as s:
        wout = k.sb(s, "w_wout", [128, 8, 1024], BF16)
        rw = k.sb(s, "w_rw", [128, 8, NE], BF16)
        rb = k.sb(s, "w_rb", [128, NE], F32)
        g1b = load_gate_rows(k, s, l, 0, "w_g1b")
        R = mk_norm_rings(k, s, "w_")
        R["gate"] = Ring([k.sb(s, "w_gt%d" % i, [128, 128], F32) for i in range(2)])
        k.R_cur = R
        xt = Ring([k.sb(s, "w_xt%d" % i, [128, 1024], F32) for i in range(5)])
        tmp = Ring([k.sb(s, "w_tmp%d" % i, [128, 1024], F32) for i in range(2)])
        hTb = Ring([k.sb(s, "w_hT%d" % i, [128, 8, 512], BF16) for i in range(2)])
        ybank = [k.ps(s, "w_y%d" % i, [128, 512]) for i in range(2)]
        yring = Ring([ybank])
        lgb = Ring([k.ps(s, "w_lg%d" % i, [128, 512]) for i in range(2)])
        wo = I["l%d_w_out" % l]
        rwd = I["l%d_router_w" % l]
        for kk in range(8):
            k.dma("pool", wout[:, kk, :], wo[kk * 128:(kk + 1) * 128, :])
            k.dma("pool", rw[:, kk, :], rwd[kk * 128:(kk + 1) * 128, :])
        k.dma("sp", rb[:], I["l%d_router_b" % l].to_broadcast([128, NE]))
        P.barrier()
        for bi, (s0, n) in enumerate(BLOCKS):
            if s0 // 128 < tiles_range[0]:
                continue
            j = 1 if bi == 0 else 0
            mixb, mtoks, mrel = get_mix(bi, s0, n)
            xs = []
            xrel = []
            lastmm = None
            for t in range(n // 128):
                ti = s0 // 128 + t
                xi, xb, xdeps = xt.get()
                d = k.dma("sp", xb[:], xsrc[ti * 128:(ti + 1) * 128, :], deps=xdeps)
                y_i, yb, ydeps = yring.get()
                for half in range(2):
                    for c in range(8):
                        lastmm = k.mm(yb[half][:], mixb[:, c, t * 128:(t + 1) * 128], wout[:, c, half * 512:(half + 1) * 512],
                                      c == 0, c == 7, deps=(mtoks + ydeps) if (c == 0 and half == 0) else ())
                tm_i, tb, tdeps = tmp.get()
                t1 = k.tt("dve", tb[:, 0:512], yb[0][:], g1b[:, j, 0:512], ALU.mult, deps=[lastmm] + tdeps)
                t2 = k.tt("dve", tb[:, 512:1024], yb[1][:], g1b[:, j, 512:1024], ALU.mult, deps=[lastmm])
                yring.release(y_i, [t2])
                t3 = k.tt("pool", xb[:], tb[:], xb[:], ALU.add, deps=[t2, d])
                tmp.release(tm_i, [t3])
                dst = k.dma("pool", xres[ti * 128:(ti + 1) * 128, :], xb[:], deps=[t3])
                xs.append((xb[:], [t3]))
                xrel.append((xi, dst))
            mrel([lastmm])
            hi, hb, hdeps = hTb.get()
            R["pT"].free[0] = R["pT"].free[0] + hdeps
            tev, tsq = k.norm_group(R, xs, m["A2"], m["modT"][:, 24:32, :], j, hb, 0)
            for xi, dst in xrel:
                xt.release(xi, tev + [dst])
            dh = k.dma("pool", h2T_d[:, :, s0:s0 + n].rearrange("c p t -> p c t"), hb[:, :, 0:n], deps=tev)
            lt = None
            for t in range(n // 128):
                ti = s0 // 128 + t
                l_i, lb, ldeps = lgb.get()
                for c in range(8):
                    lt = k.mm(lb[:, 0:NE], hb[:, c, t * 128:(t + 1) * 128], rw[:, c, :], c == 0, c == 7,
                              deps=(tev + ldeps) if c == 0 else ())
                t1, t9 = k.gating(R, lb[:, 0:NE], ti, k.Gall, rb, [lt])
                lgb.release(l_i, [t1])
            hTb.release(hi, [lt, dh])
        P.barrier()


def phase_moe(k, I, l, xres, h2T_d, groups, final_out=None):
    nc, P = k.nc, k.P
    with ExitStack() as s:
        GMAX = max(g[1] - g[0] for g in groups)
        hTg = k.sb(s, "m_hT", [128, 8, GMAX * 128], BF16)
        yacc = k.sb(s, "m_yacc", [128, GMAX, 1024], F32)
        wgu = Ring([k.sb(s, "m_wgu%d" % i, [128, 8, 2048], BF16) for i in range(2)])
        wd = Ring([k.sb(s, "m_wd%d" % i, [128, 8, 1024], BF16) for i in range(2)])
        aT = Ring([k.sb(s, "m_aT%d" % i, [128, 8, 512], BF16) for i in range(2)])
        tg = Ring([k.sb(s, "m_tg%d" % i, [128, 4, 512], F32) for i in range(1)])
        bgu = k.sb(s, "m_bgu", [128, NE, 8, 2], F32)
        bdn = k.sb(s, "m_bdn", [NE, 1024], F32)
        GTs = Ring([k.sb(s, "m_GT%d" % i, [NE, 128], F32) for i in range(2)])
        g2b = load_gate_rows(k, s, l, 1, "m_g2b")
        xt = Ring([k.sb(s, "m_xt%d" % i, [128, 1024], F32) for i in range(2)])
        glb = Ring([[k.ps(s, "m_gl%d_%d" % (i, c), [128, 512]) for c in range(2)] for i in range(2)])
        yb = Ring([[k.ps(s, "m_y%d_%d" % (i, c), [128, 512]) for c in range(2)] for i in range(2)])
        fng = None
        if final_out is not None:
            fng = k.sb(s, "m_fng", [128, 1024], F32)
            k.dma("sp", fng[:], I["final_norm_g"].to_broadcast([128, 1024]))
            fsq = k.sb(s, "m_fsq", [128, 4], F32)
            fjunk = k.sb(s, "m_fjunk", [128, 1024], BF16)
        k.dma("sp", bgu[:], I["l%d_b_gu_fm" % l])
        k.dma("sp", bdn[:], I["l%d_b_down" % l])
        P.barrier()
        k.ts("dve", bgu[:, :, :, 1], bgu[:, :, :, 1], 1.0, None, ALU.add)
        P.barrier()
        w_gu = I["l%d_w_gu" % l]
        w_dn = I["l%d_w_down" % l]

        def load_w(e, first=False):
            gi, gbuf, gdeps = wgu.get()
            di, dbuf, ddeps = wd.get()
            tk = []
            for kk in range(8):
                tk.append(k.dma("pool", gbuf[:, kk, :], w_gu[e, kk * 128:(kk + 1) * 128, :], deps=gdeps if kk == 0 else ()))
            for kk in range(8):
                tk.append(k.dma("pool", dbuf[:, kk, :], w_dn[e, kk * 128:(kk + 1) * 128, :], deps=ddeps if kk == 0 else ()))
            return (gi, gbuf, di, dbuf, tk)

        hT_last = []
        fin_last = []
        fq_last = []
        for gidx, (ta, tb_) in enumerate(groups):
            ng = tb_ - ta
            c0g = ta * 128
            dh = k.dma("sp", hTg[:, :, 0:ng * 128], h2T_d[:, :, c0g:c0g + ng * 128].rearrange("c p t -> p c t"), deps=hT_last)
            nxt = load_w(0)
            tinit = []
            for ti in range(ng):
                g_i, gp, gdeps = glb.get()
                ttr = k.tr(gp[0][0:NE, 0:128], k.Gall[:, ta + ti, :], k.identf[:], deps=gdeps)
                gt_i, gts, gtdeps = GTs.get()
                tc = k.cp("act", gts[:], gp[0][0:NE, 0:128], deps=[ttr] + gtdeps)
                glb.release(g_i, [tc])
                y_i, ybk, ydeps = yb.get()
                tm = None
                for half in range(2):
                    tm = k.mm(ybk[half][:], gts[:], bdn[:, half * 512:(half + 1) * 512], True, True,
                              deps=([tc] + ydeps) if half == 0 else ())
                GTs.release(gt_i, [tm])
                t0 = k.cp("act", yacc[:, ti, 0:512], ybk[0][:], deps=[tm] + fin_last)
                t1 = k.cp("dve", yacc[:, ti, 512:1024], ybk[1][:], deps=[tm] + fin_last)
                yb.release(y_i, [t0, t1])
                tinit += [t0, t1]
            tinit = tinit[-2:]
            subs = []
            t_ = 0
            if l == 0 and gidx == 0:
                subs.append((0, 2)); t_ = 2
            while t_ < ng:
                subs.append((t_, min(4, ng - t_))); t_ += min(4, ng - t_)
            yacc_tok = {}
            for e in range(NE):
                gi, gbuf, di, dbuf, wtok = nxt
                if e + 1 < NE:
                    nxt = load_w(e + 1)
                lastpe = None
                for (st, ntl) in subs:
                    n = ntl * 128
                    cc = st * 128
                    a_i, ab, adeps = aT.get()
                    for jf in range(8):
                        p_i, pr, pdeps = glb.get()
                        first = [dh] + wtok if (lastpe is None and jf == 0) else []
                        for kk in range(8):
                            k.mm(pr[0][:, 0:n], gbuf[:, kk, jf * 256:(jf + 1) * 256:2], hTg[:, kk, cc:cc + n], kk == 0, kk == 7,
                                 deps=(first + pdeps) if kk == 0 else (), sig=False)
                        tmm = None
                        for kk in range(8):
                            tmm = k.mm(pr[1][:, 0:n], gbuf[:, kk, jf * 256 + 1:(jf + 1) * 256:2], hTg[:, kk, cc:cc + n], kk == 0, kk == 7)
                        lastpe = tmm
                        tg_i, tgb, tgdeps = tg.get()
                        e1 = k.ts("dve", tgb[:, 0, 0:n], pr[0][:, 0:n], bgu[:, e, jf, 0:1], 7.0, ALU.add, ALU.min, deps=[tmm] + tgdeps)
                        e2 = k.act(tgb[:, 1, 0:n], tgb[:, 0, 0:n], AF.Sigmoid, deps=[e1], scale=1.702)
                        e3 = k.ts("dve", tgb[:, 2, 0:n], pr[1][:, 0:n], bgu[:, e, jf, 1:2], 8.0, ALU.add, ALU.min, deps=[tmm])
                        glb.release(p_i, [e3])
                        e4 = k.stt("dve", tgb[:, 3, 0:n], tgb[:, 2, 0:n], -6.0, tgb[:, 0, 0:n], ALU.max, ALU.mult, deps=[e3])
                        e5 = k.tt("dve", ab[:, jf, 0:n], tgb[:, 3, 0:n], tgb[:, 1, 0:n], ALU.mult, deps=[e4, e2] + (adeps if jf == 0 else []))
                        tg.release(tg_i, [e5])
                    for t in range(ntl):
                        ti = st + t
                        y_i, ybk, ydeps = yb.get()
                        tm = None
                        for half in range(2):
                            for jf in range(8):
                                tm = k.mm(ybk[half][:], ab[:, jf, t * 128:(t + 1) * 128], dbuf[:, jf, half * 512:(half + 1) * 512],
                                          jf == 0, jf == 7, deps=([e5] + ydeps) if (jf == 0 and half == 0) else ())
                        lastpe = tm
                        prev = yacc_tok.get(ti, tinit)
                        u0 = k.stt("dve", yacc[:, ti, 0:512], ybk[0][:], k.Gall[:, ta + ti, e:e + 1], yacc[:, ti, 0:512],
                                   ALU.mult, ALU.add, deps=[tm] + prev)
                        u1 = k.stt("dve", yacc[:, ti, 512:1024], ybk[1][:], k.Gall[:, ta + ti, e:e + 1], yacc[:, ti, 512:1024],
                                   ALU.mult, ALU.add, deps=[tm])
                        yb.release(y_i, [u1])
                        yacc_tok[ti] = [u1]
                    aT.release(a_i, [lastpe])
                wgu.release(gi, [lastpe])
                wd.release(di, [lastpe])
                hT_last = [lastpe]
            for ti in range(ng):
                tile = ta + ti
                j = 1 if tile < 2 else 0
                xi, xb, xdeps = xt.get()
                d = k.dma("sp", xb[:], xres[tile * 128:(tile + 1) * 128, :], deps=xdeps)
                f1 = k.tt("dve", yacc[:, ti, :], yacc[:, ti, :], g2b[:, j, :], ALU.mult, deps=yacc_tok[ti])
                f2 = k.tt("dve", xb[:], xb[:], yacc[:, ti, :], ALU.add, deps=[f1, d])
                fin_last = [f2]
                if final_out is None:
                    dd = k.dma("sp", xres[tile * 128:(tile + 1) * 128, :], xb[:], deps=[f2])
                else:
                    q1 = k.act(fjunk[:], xb[:], AF.Square, deps=[f2] + fq_last, accum_out=fsq[:, 0:1])
                    q2 = k.act(fsq[:, 1:2], fsq[:, 0:1], AF.Sqrt, deps=[q1], scale=1.0 / D, bias=k.eps[:])
                    q3 = k.recip(fsq[:, 2:3], fsq[:, 1:2], deps=[q2])
                    q4 = k.stt("dve", xb[:], xb[:], fsq[:, 2:3], fng[:], ALU.mult, ALU.mult, deps=[q3])
                    dd = k.dma("sp", final_out[(tile - 2) * 128:(tile - 1) * 128, :], xb[:], deps=[q4])
                    fq_last = [q1, q4]
                xt.release(xi, [dd])
            P.barrier()


def _bf(a):
    return np.asarray(a, np.float32).astype(ml_dtypes.bfloat16)


def make_consts():
    C = {}
    C["c_identb"] = _bf(np.eye(128))
    C["c_identf"] = np.eye(128, dtype=np.float32)
    pos = np.arange(TL)
    row = (pos // 64).astype(np.float32)
    col = (pos % 64).astype(np.float32)
    inv = (np.float32(10000.0) ** (-np.arange(0, 32, 2, dtype=np.float32) / np.float32(32))).astype(np.float32)
    ar = row[:, None] * inv[None, :]
    ac = col[:, None] * inv[None, :]
    ang = np.concatenate([ar, ar, ac, ac], -1).astype(np.float32)
    cosT = np.ones((64, T), np.float32)
    sinT = np.zeros((64, T), np.float32)
    cosT[:, TC:] = np.cos(ang).T
    sinT[:, TC:] = np.sin(ang).T
    C["c_cos"] = cosT
    C["c_sin"] = sinT
    jl = np.outer(np.arange(64), np.arange(64)) % 64
    c64 = np.cos(2 * np.pi * jl / 64.0) / 8.0
    s64 = np.sin(2 * np.pi * jl / 64.0) / 8.0
    cs = np.zeros((128, 256))
    for g in range(2):
        cs[g * 64:(g + 1) * 64, g * 64:(g + 1) * 64] = c64
        cs[g * 64:(g + 1) * 64, 128 + g * 64:128 + (g + 1) * 64] = s64
    C["c_cs64"] = _bf(cs)

    def dft(N):
        m = (np.arange(N, dtype=np.int64)[:, None] * np.arange(N, dtype=np.int64)[None, :]) % N
        a = 2 * np.pi * m.astype(np.float64) / N
        out = np.empty((N, 2, N), dtype=ml_dtypes.bfloat16)
        out[:, 0, :] = (np.cos(a) / np.sqrt(N)).astype(np.float32).astype(ml_dtypes.bfloat16)
        out[:, 1, :] = (-np.sin(a) / np.sqrt(N)).astype(np.float32).astype(ml_dtypes.bfloat16)
        return out
    jj = np.arange(128)[:, None]
    ii = np.arange(128)[None, :]
    same = (jj // 64) == (ii // 64)
    tri = np.zeros((128, 4, 128), np.float32)
    tri[:, 0, :] = np.where(same & (jj <= ii), -1.0 / 16, 0.0)
    tri[:, 1, :] = np.where(same & (jj >= ii), -1.0 / 16, 0.0)
    tri[:, 2, :] = np.where(same & (jj > ii), -1.0 / 16, 0.0)
    tri[:, 3, :] = np.where(same & (jj < ii), -1.0 / 16, 0.0)
    C["c_tri"] = tri
    C["c_lnS"] = np.full((128, 1), np.log(128.0 ** -0.5), np.float32)
    j6 = np.arange(64)[:, None]
    i6 = np.arange(64)[None, :]
    mk = np.zeros((64, 2, 256), np.float32)
    for h in range(4):
        mk[:, 0, h * 64:(h + 1) * 64] = (j6 <= i6)
        mk[:, 1, h * 64:(h + 1) * 64] = (j6 >= i6)
    C["c_mask"] = _bf(mk)
    C["c_dft256"] = dft(256)
    C["c_dft4096"] = dft(4096)
    return C


_CONSTS = None

IN_SPECS = None


def input_specs():
    sp = {
        "xin": ([T, D], F32), "cfm": ([128, 8, 2], F32), "final_norm_g": ([1, D], F32),
        "c_identb": ([128, 128], BF16), "c_identf": ([128, 128], F32), "c_cos": ([64, T], F32), "c_sin": ([64, T], F32),
        "c_cs64": ([128, 256], BF16), "c_dft256": ([256, 2, 256], BF16), "c_dft4096": ([4096, 2, 4096], BF16),
    }
    for l in range(2):
        p = "l%d_" % l
        sp[p + "mod_w"] = ([D, 6 * D], F32)
        sp[p + "mod_b"] = ([1, 6 * D], F32)
        sp[p + "mod_b_fm"] = ([128, 48], F32)
        sp[p + "norm1_g_fm"] = ([128, 8], F32)
        sp[p + "norm2_g_fm"] = ([128, 8], F32)
        sp[p + "w_out"] = ([D, D], F32)
        sp[p + "router_w"] = ([D, NE], F32)
        sp[p + "router_b"] = ([1, NE], F32)
        sp[p + "w_gu"] = ([NE, D, 2 * D], F32)
        sp[p + "b_gu_fm"] = ([128, NE, 8, 2], F32)
        sp[p + "w_down"] = ([NE, D, D], F32)
        sp[p + "b_down"] = ([NE, D], F32)
    sp["l1_w_in"] = ([D, 3104], F32)
    sp["l1_wz"] = ([33, 1024], F32)
    sp["l1_gnorm_g"] = ([1, 256], F32)
    sp["c_tri"] = ([128, 4, 128], F32)
    sp["c_lnS"] = ([128, 1], F32)
    sp["c_mask"] = ([64, 2, 256], BF16)
    sp["l0_w_in"] = ([D, 960], F32)
    sp["l0_q_norm_g_fm"] = ([128, 3], F32)
    sp["l0_w_q_up"] = ([384, 1152], F32)
    sp["l0_kv_norm_g_fm"] = ([128, 2], F32)
    sp["l0_w_kv_up"] = ([256, 1536], F32)
    return sp


ALL_INPUT_NAMES = (
    "x", "c", "ctx", "c_ctx", "final_norm_g",
    "l0_mod_w", "l0_mod_b", "l0_norm1_g", "l0_w_in", "l0_q_norm_g", "l0_w_q_up", "l0_kv_norm_g", "l0_w_kv_up",
    "l0_w_out", "l0_norm2_g", "l0_router_w", "l0_router_b", "l0_w_gu", "l0_b_gu", "l0_w_down", "l0_b_down",
    "l1_mod_w", "l1_mod_b", "l1_norm1_g", "l1_w_in", "l1_w_gk_fwd", "l1_b_gk_fwd", "l1_w_gk_bwd", "l1_b_gk_bwd",
    "l1_gnorm_g", "l1_w_out", "l1_norm2_g", "l1_router_w", "l1_router_b", "l1_w_gu", "l1_b_gu", "l1_w_down",
    "l1_b_down",
)


def host_inputs(inputs, b):
    global _CONSTS
    if _CONSTS is None:
        _CONSTS = make_consts()
    missing = [n for n in ALL_INPUT_NAMES if n not in inputs]
    assert not missing, missing
    g = lambda n: np.asarray(inputs[n], np.float32)
    m = dict(_CONSTS)
    m["xin"] = np.ascontiguousarray(np.concatenate([g("ctx")[b], g("x")[b]], 0))
    m["cfm"] = np.ascontiguousarray(np.stack([fm(g("c")[b]), fm(g("c_ctx"))], -1))
    m["final_norm_g"] = g("final_norm_g").reshape(1, D)
    for l in range(2):
        p = "l%d_" % l
        m[p + "mod_w"] = g(p + "mod_w")
        m[p + "mod_b"] = g(p + "mod_b").reshape(1, -1)
        m[p + "mod_b_fm"] = fm(g(p + "mod_b"))
        m[p + "norm1_g_fm"] = fm(g(p + "norm1_g"))
        m[p + "norm2_g_fm"] = fm(g(p + "norm2_g"))
        m[p + "w_out"] = g(p + "w_out")
        m[p + "router_w"] = g(p + "router_w")
        m[p + "router_b"] = g(p + "router_b").reshape(1, NE)
        m[p + "w_gu"] = g(p + "w_gu")
        m[p + "b_gu_fm"] = np.ascontiguousarray(g(p + "b_gu").reshape(NE, 8, 128, 2).transpose(2, 0, 1, 3))
        m[p + "w_down"] = g(p + "w_down")
        m[p + "b_down"] = g(p + "b_down")
    m["l1_w_in"] = g("l1_w_in")
    wz = np.zeros((33, 1024), np.float32)
    wz[0:16, 0:512] = g("l1_w_gk_fwd")
    wz[16:32, 512:1024] = g("l1_w_gk_bwd")
    wz[32, 0:512] = g("l1_b_gk_fwd")
    wz[32, 512:1024] = g("l1_b_gk_bwd")
    m["l1_wz"] = wz
    m["l1_gnorm_g"] = g("l1_gnorm_g").reshape(1, 256)
    m["l0_w_in"] = g("l0_w_in")
    m["l0_q_norm_g_fm"] = fm(g("l0_q_norm_g"))
    m["l0_w_q_up"] = g("l0_w_q_up")
    m["l0_kv_norm_g_fm"] = fm(g("l0_kv_norm_g"))
    m["l0_w_kv_up"] = g("l0_w_kv_up")
    return m


L0_GROUPS = [(0, 6), (6, 13), (13, 20), (20, 27), (27, 34)]
L1_GROUPS = [(2, 9), (9, 16), (16, 22), (22, 28), (28, 34)]


def build():
    nc = bass.Bass("TRN2", target_bir_lowering=False)
    I = {}
    for name, (shape, dt) in input_specs().items():
        if L1_ONLY and (name.startswith("l0_") or name in ("c_dft4096", "c_dft256", "c_cs64", "c_cos", "c_sin", "xin")
                        or name in ("l1_w_gu", "l1_w_down", "l1_b_gu_fm", "l1_b_down")):
            continue
        I[name] = nc.dram_tensor(name, shape, dt, kind="ExternalInput").ap()
    if L1_ONLY:
        xres = nc.dram_tensor("xres_in", [T, D], F32, kind="ExternalInput").ap()
    else:
        out = nc.dram_tensor("out", [TL, D], F32, kind="ExternalOutput").ap()
        xres = nc.dram_tensor("xres", [T, D], F32, kind="Internal").ap()
    mix_d = nc.dram_tensor("mix_d", [8, 128, T], BF16, kind="Internal").ap()
    h2T_d = nc.dram_tensor("h2T_d", [8, 128, T], BF16, kind="Internal").ap()
    dbg = {}
    with ExitStack() as es:
        k = K(nc, es)
        k.mod = {}
        P = k.P
        stop = STOP_AFTER
        dbg_src = None

        def body():
            nonlocal dbg_src
            phase_setup(k, I)
            if stop == "setup":
                dbg_src = I["xin"]; return
            if not L1_ONLY:
                phase_adaln(k, I, 0)
            phase_adaln(k, I, 1)
            if stop == "adaln":
                dbg_src = I["xin"]; return
            if not L1_ONLY:
                body_l0()
                if dbg_src is not None:
                    return
            body_l1()

        def body_l0():
            nonlocal dbg_src
            with ExitStack() as s0:
                cosT = k.sb(s0, "cosT", [64, T], F32)
                sinT = k.sb(s0, "sinT", [64, T], F32)
                cqn = k.sb(s0, "cqn", [128, 3, T], BF16)
                ckvn = k.sb(s0, "ckvn", [128, 2, T], BF16)
                krT = k.sb(s0, "krT", [64, T], BF16)
                k.dma("sp", cosT[:], I["c_cos"])
                k.dma("sp", sinT[:], I["c_sin"])
                with ExitStack() as s1:
                    UT = k.sb(s1, "UT", [128, 2, T], BF16)
                    phase_l0_inproj(k, I, cqn, ckvn, krT, UT, cosT, sinT)
                    if stop == "inproj":
                        dbg_src = I["xin"]; return
                    phase_l0_fourier(k, I, UT, mix_d)
                    if stop == "fourier":
                        dbg_src = I["xin"]; return
                phase_l0_attn(k, I, cqn, ckvn, krT, cosT, sinT, mix_d)
                if stop == "attn":
                    dbg_src = I["xin"]; return
            with ExitStack() as s2:
                mixr = Ring([k.sb(s2, "mixb%d" % i, [128, 8, 512], BF16) for i in range(2)])

                def get_mix(bi, s0_, n):
                    i, buf, deps = mixr.get()
                    d = k.dma("sp", buf[:, :, 0:n], mix_d[:, :, s0_:s0_ + n].rearrange("c p t -> p c t"), deps=deps)
                    return buf, [d], (lambda toks, i=i: mixr.release(i, toks))
                phase_wout(k, I, 0, get_mix, I["xin"], xres, h2T_d, (0, NT))
            if stop == "l0_wout":
                dbg_src = xres; return
            phase_moe(k, I, 0, xres, h2T_d, L0_GROUPS)
            if stop == "l0":
                dbg_src = xres; return

        def body_l1():
            nonlocal dbg_src
            with ExitStack() as s3:
                S1 = dict(
                    QD=nc.dram_tensor("QD_d", [8, 128, T], BF16, kind="Internal").ap(),
                    KD=nc.dram_tensor("KD_d", [8, 128, T], BF16, kind="Internal").ap(),
                    KK=nc.dram_tensor("KK_d", [T, 2, 512], BF16, kind="Internal").ap(),
                    V=nc.dram_tensor("V_d", [T, 1024], BF16, kind="Internal").ap(),
                    GO=nc.dram_tensor("GO_d", [T, 1024], BF16, kind="Internal").ap(),
                    O=nc.dram_tensor("O_d", [2, TL, 1024], F32, kind="Internal").ap(),
                    EL=k.sb(s3, "EL", [128, 8, 68], F32))
                phase_l1_prep(k, I, xres, S1)
                phase_l1_scan(k, I, S1)
                if stop == "l1_scan":
                    dbg_src = S1["O"][0]; return
                with ExitStack() as s4:
                    gm = l1_get_mix_factory(k, I, s4, S1)
                    k.P.barrier()
                    phase_wout(k, I, 1, gm, xres, xres, h2T_d, (2, NT))
            if stop == "l1_wout":
                dbg_src = xres; return
            phase_moe(k, I, 1, xres, h2T_d, L1_GROUPS, final_out=out)

        body()
        if dbg_src is not None:
            nrows = dbg_src.shape[0]
            dbg_out = nc.dram_tensor("dbg", [nrows, D], F32, kind="ExternalOutput").ap()
            copy_dram(k, dbg_out, dbg_src, nrows // 128)
        P.barrier()
        P.emit()
    return nc


def copy_dram(k, dst, src, ntiles=NT):
    with ExitStack() as s:
        bufs = Ring([k.sb(s, "cpb%d" % i, [128, 1024], F32) for i in range(2)])
        for ti in range(ntiles):
            i, b, deps = bufs.get()
            d = k.dma("sp", b[:], src[ti * 128:(ti + 1) * 128, :], deps=deps)
            d2 = k.dma("sp", dst[ti * 128:(ti + 1) * 128, :], b[:], deps=[d])
            bufs.release(i, [d2])
        k.P.barrier()


def phase_l1_prep(k, I, xres, S1):
    nc, P = k.nc, k.P
    m = k.mod[1]
    QD_d, KD_d, KK_d, V_d, GO_d, EL = S1["QD"], S1["KD"], S1["KK"], S1["V"], S1["GO"], S1["EL"]
    with ExitStack() as s:
        win = k.sb(s, "p1_win", [128, 8, 3104], BF16)
        wz = k.sb(s, "p1_wz", [33, 1024], F32)
        tri = k.sb(s, "p1_tri", [128, 4, 128], F32)
        lnS = k.sb(s, "p1_lnS", [128, 1], F32)
        R = mk_norm_rings(k, s, "p1_")
        xt = Ring([k.sb(s, "p1_xt%d" % i, [128, 1024], F32) for i in range(4)])
        hTb = Ring([k.sb(s, "p1_hT%d" % i, [128, 8, 512], BF16) for i in range(2)])
        qk32 = k.sb(s, "p1_qk32", [128, 8, 512], F32)
        gdx = k.sb(s, "p1_gdx", [33, 512], F32)
        qdb = Ring([k.sb(s, "p1_qdb%d" % i, [128, 8, 512], BF16) for i in range(2)])
        kdb = Ring([k.sb(s, "p1_kdb%d" % i, [128, 8, 512], BF16) for i in range(2)])
        spb = k.sb(s, "p1_sp", [128, 1024], F32)
        eD = k.sb(s, "p1_eD", [128, 1024], F32)
        ec = Ring([k.sb(s, "p1_ec%d" % i, [128, 2, 128], F32) for i in range(2)])
        tmo = Ring([k.sb(s, "p1_tmo%d" % i, [128, 3, 1024], BF16) for i in range(2)])
        banks = Ring([k.ps(s, "p1_b%d" % i, [128, 512]) for i in range(4)])
        w_in = I["l1_w_in"]
        for kk in range(8):
            k.dma("pool", win[:, kk, :], w_in[kk * 128:(kk + 1) * 128, :])
        k.dma("sp", wz[:], I["l1_wz"])
        k.dma("sp", tri[:], I["c_tri"])
        k.dma("sp", lnS[:], I["c_lnS"])
        k.memset("dve", gdx[32:33, :], 1.0)
        P.barrier()
        eD_last = []
        qk_last = []
        for bi, (s0, n) in enumerate(BLOCKS):
            j = 1 if bi == 0 else 0
            ntl = n // 128
            xs, xrel = [], []
            for t in range(ntl):
                xi, xb, xdeps = xt.get()
                d = k.dma("sp", xb[:], xres[s0 + t * 128:s0 + (t + 1) * 128, :], deps=xdeps)
                xs.append((xb[:], [d])); xrel.append(xi)
            hi, hb, hdeps = hTb.get()
            R["pT"].free[0] = R["pT"].free[0] + hdeps
            tev, _ = k.norm_group(R, xs, m["A1"], m["modT"], j, hb, 0)
            for xi in xrel:
                xt.release(xi, tev)

            def fmproj(c0, M, extra):
                b_i, bank, bdeps = banks.get()
                t = None
                for kk in range(8):
                    t = k.mm(bank[0:M, 0:n], win[:, kk, c0:c0 + M], hb[:, kk, 0:n], kk == 0, kk == 7,
                             deps=(extra + bdeps) if kk == 0 else ())
                return b_i, bank, t
            tqk = []
            for c in range(8):
                b_i, bank, t = fmproj(c * 128, 128, tev if c == 0 else [])
                te = k.cp("act" if c % 2 else "dve", qk32[:, c, 0:n], bank[:, 0:n], deps=[t] + qk_last)
                banks.release(b_i, [te]); tqk.append(te)
            b_i, bank, t = fmproj(3072, 32, [])
            tgd = k.cp("dve", gdx[0:32, 0:n], bank[0:32, 0:n], deps=[t])
            banks.release(b_i, [tgd])
            q_i, qb, qdeps = qdb.get()
            k_i, kb, kdeps = kdb.get()
            lastq = lastk = None
            lastpe = None
            for t in range(ntl):
                ti = s0 // 128 + t
                tc = slice(t * 128, (t + 1) * 128)
                tsp = []
                for half in range(2):
                    b_i, bank, bdeps = banks.get()
                    tz = k.mm(bank[:], gdx[0:33, tc], wz[0:33, half * 512:(half + 1) * 512], True, True, deps=[tgd] + bdeps)
                    t1 = k.act(spb[:, half * 512:(half + 1) * 512], bank[:], AF.Exp, deps=[tz], scale=-1.0)
                    banks.release(b_i, [t1])
                    t2 = k.act(spb[:, half * 512:(half + 1) * 512], spb[:, half * 512:(half + 1) * 512], AF.Ln, deps=[t1], bias=1.0)
                    tsp.append(t2)
                tD = []
                for dr in range(2):
                    b_i, bank, bdeps = banks.get()
                    tm = k.mm(bank[:], tri[:, 2 + dr, :], spb[:, dr * 512:(dr + 1) * 512], True, True, deps=tsp + bdeps)
                    te = k.act(eD[:, dr * 512:(dr + 1) * 512], bank[:], AF.Exp, deps=[tm] + eD_last)
                    banks.release(b_i, [te]); tD.append(te)
                o_i, ob, odeps = tmo.get()
                b_i, bank, bdeps = banks.get()
                tk_ = None
                for kk in range(8):
                    tk_ = k.mm(bank[:], hb[:, kk, tc], win[:, kk, 512:1024], kk == 0, kk == 7, deps=bdeps if kk == 0 else ())
                w1 = k.tt("dve", ob[:, 0, 0:512], bank[:], eD[:, 0:512], ALU.mult, deps=[tk_, tD[0]] + odeps)
                w2 = k.tt("dve", ob[:, 0, 512:1024], bank[:], eD[:, 512:1024], ALU.mult, deps=[tD[1]])
                eD_last = [w2]
                banks.release(b_i, [w2])
                tl_ = []
                for q4 in range(4):
                    b_i, bank, bdeps = banks.get()
                    c0 = 1024 + q4 * 512
                    tm = None
                    for kk in range(8):
                        tm = k.mm(bank[:], hb[:, kk, tc], win[:, kk, c0:c0 + 512], kk == 0, kk == 7, deps=bdeps if kk == 0 else ())
                    if q4 < 2:
                        te = k.cp("dve", ob[:, 1, q4 * 512:(q4 + 1) * 512], bank[:], deps=[tm])
                    else:
                        te = k.act(ob[:, 2, (q4 - 2) * 512:(q4 - 1) * 512], bank[:], AF.Silu, deps=[tm])
                    banks.release(b_i, [te]); tl_.append(te)
                    lastpe = tm
                d1 = k.dma("pool", KK_d[ti * 128:(ti + 1) * 128, :, :], ob[:, 0, :].rearrange("p (a d) -> p a d", a=2), deps=[w1, w2])
                d2 = k.dma("pool", V_d[ti * 128:(ti + 1) * 128, :], ob[:, 1, :], deps=tl_[0:2])
                d3 = k.dma("pool", GO_d[ti * 128:(ti + 1) * 128, :], ob[:, 2, :], deps=tl_[2:4])
                tmo.release(o_i, [d1, d2, d3])
                for dr in range(2):
                    for h in range(4):
                        b_i, bank, bdeps = banks.get()
                        tm = k.mm(bank[:, 0:128], spb[:, dr * 512 + h * 128:dr * 512 + (h + 1) * 128], tri[:, dr, :], True, True,
                                  deps=tsp + bdeps)
                        e_i, eb, edeps = ec.get()
                        a1 = k.act(eb[:, 0, :], bank[:, 0:128], AF.Exp, deps=[tm] + edeps, bias=lnS[:], scale=1.0)
                        a2 = k.act(eb[:, 1, :], bank[:, 0:128], AF.Exp, deps=[tm], scale=-1.0)
                        lc = 63 if dr == 0 else 0
                        a3 = k.act(EL[:, dr * 4 + h, 2 * ti:2 * ti + 2], bank[:, lc:128:64], AF.Exp, deps=[tm])
                        banks.release(b_i, [a3])
                        lastq = k.tt("dve", qb[:, dr * 4 + h, tc], qk32[:, h, tc], eb[:, 0, :], ALU.mult,
                                     deps=[a1, tqk[h]] + (qdeps if (t == 0 and dr == 0 and h == 0) else []))
                        lastk = k.tt("dve", kb[:, dr * 4 + h, tc], qk32[:, 4 + h, tc], eb[:, 1, :], ALU.mult,
                                     deps=[a2, tqk[4 + h]] + (kdeps if (t == 0 and dr == 0 and h == 0) else []))
                        ec.release(e_i, [lastk])
            dq = k.dma("pool", QD_d[:, :, s0:s0 + n].rearrange("c p t -> p c t"), qb[:, :, 0:n], deps=[lastq])
            dk = k.dma("pool", KD_d[:, :, s0:s0 + n].rearrange("c p t -> p c t"), kb[:, :, 0:n], deps=[lastk])
            qdb.release(q_i, [dq]); kdb.release(k_i, [dk])
            hTb.release(hi, [lastpe])
            qk_last = [lastq, lastk]
        P.barrier()


def phase_l1_scan(k, I, S1):
    nc, P = k.nc, k.P
    QD_d, KD_d, KK_d, V_d, O_d, EL = S1["QD"], S1["KD"], S1["KK"], S1["V"], S1["O"], S1["EL"]
    with ExitStack() as s:
        St = k.sb(s, "sc_S", [128, 8, 256], F32)
        Sb = k.sb(s, "sc_Sb", [128, 8, 256], BF16)
        msk = k.sb(s, "sc_msk", [64, 2, 256], BF16)
        inr = Ring([dict(qd=k.sb(s, "sc_qd%d" % i, [128, 4, 64], BF16), kd=k.sb(s, "sc_kd%d" % i, [128, 4, 64], BF16),
                         kk=k.sb(s, "sc_kk%d" % i, [64, 512], BF16), v=k.sb(s, "sc_v%d" % i, [64, 1024], BF16)) for i in range(4)])
        scm = Ring([k.sb(s, "sc_scm%d" % i, [64, 256], BF16) for i in range(2)])
        osb = Ring([k.sb(s, "sc_o%d" % i, [64, 1024], F32) for i in range(3)])
        scb = Ring([k.ps(s, "sc_ps%d" % i, [128, 512]) for i in range(2)])
        obk = Ring([[k.ps(s, "sc_po%d_%d" % (i, c), [128, 512]) for c in range(2)] for i in range(2)])
        kvb = Ring([k.ps(s, "sc_pk%d" % i, [128, 512]) for i in range(2)])
        k.dma("sp", msk[:], I["c_mask"])
        k.memset("dve", St[:], 0.0)
        k.memset("pool", Sb[:], 0.0)
        P.barrier()
        stok = [[None] * 4 for _ in range(2)]
        sdve = [[None] * 4 for _ in range(2)]
        for step in range(68):
            for dr in range(2):
                c = step if dr == 0 else ((3 - step) if step < 4 else (71 - step))
                lat = c >= 4
                i_i, ib, ideps = inr.get()
                cs = slice(c * 64, (c + 1) * 64)
                dl = [k.dma("sp", ib["kk"][:], KK_d[cs, dr, :], deps=ideps),
                      k.dma("sp", ib["v"][:], V_d[cs, :], deps=ideps)]
                if lat:
                    dl.append(k.dma("sp", ib["qd"][:], QD_d[dr * 4:(dr + 1) * 4, :, cs].rearrange("h p t -> p h t"), deps=ideps))
                    dl.append(k.dma("sp", ib["kd"][:], KD_d[dr * 4:(dr + 1) * 4, :, cs].rearrange("h p t -> p h t"), deps=ideps))
                lastpe = None
                if lat:
                    s_i, sbk, sdeps = scb.get()
                    t = None
                    for h in range(4):
                        t = k.mm(sbk[0:64, h * 64:(h + 1) * 64], ib["kd"][:, h, :], ib["qd"][:, h, :], True, True,
                                 deps=(dl + sdeps) if h == 0 else (), sig=(h == 3))
                    m_i, mb, mdeps = scm.get()
                    tmk = k.tt("dve", mb[:], sbk[0:64, 0:256], msk[:, dr, :], ALU.mult, deps=[t] + mdeps)
                    scb.release(s_i, [tmk])
                    o_i, ob, odeps = obk.get()
                    for h in range(4):
                        oo = ob[h // 2][0:64, (h % 2) * 256:(h % 2) * 256 + 256]
                        k.mm(oo, ib["qd"][:, h, :], Sb[:, dr * 4 + h, :], True, False,
                             deps=([stok[dr][h]] if stok[dr][h] else []) + (odeps if h == 0 else []), sig=False)
                        lastpe = k.mm(oo, mb[:, h * 64:(h + 1) * 64], ib["v"][0:64, h * 256:(h + 1) * 256], False, True,
                                      deps=[tmk] if h == 0 else ())
                    scm.release(m_i, [lastpe])
                    os_i, osbuf, osdeps = osb.get()
                    e0 = k.cp("act", osbuf[:, 0:512], ob[0][0:64, :], deps=[lastpe] + osdeps)
                    e1 = k.cp("dve", osbuf[:, 512:1024], ob[1][0:64, :], deps=[lastpe] + osdeps)
                    obk.release(o_i, [e0, e1])
                    dd = k.dma("pool", O_d[dr, (c - 4) * 64:(c - 3) * 64, :], osbuf[:], deps=[e0, e1])
                    osb.release(os_i, [dd])
                for hp in range(2):
                    kv_i, kvbank, kvdeps = kvb.get()
                    t = None
                    for hh in range(2):
                        h = hp * 2 + hh
                        t = k.mm(kvbank[:, hh * 256:(hh + 1) * 256], ib["kk"][0:64, h * 128:(h + 1) * 128],
                                 ib["v"][0:64, h * 256:(h + 1) * 256], True, True,
                                 deps=(dl + kvdeps) if hh == 0 else (), sig=(hh == 1))
                    lastpe = t
                    ups = []
                    for hh in range(2):
                        h = hp * 2 + hh
                        u = k.stt("dve", St[:, dr * 4 + h, :], St[:, dr * 4 + h, :], EL[:, dr * 4 + h, c:c + 1],
                                  kvbank[:, hh * 256:(hh + 1) * 256], ALU.mult, ALU.add,
                                  deps=[t] + ([sdve[dr][h]] if sdve[dr][h] else []))
                        sdve[dr][h] = u
                        ups.append(u)
                        stok[dr][h] = k.cp("act", Sb[:, dr * 4 + h, :], St[:, dr * 4 + h, :], deps=[u, lastpe])
                    kvb.release(kv_i, ups)
                inr.release(i_i, [lastpe])
        P.barrier()


def l1_get_mix_factory(k, I, s, S1):
    O_d, GO_d = S1["O"], S1["GO"]
    gng = k.sb(s, "g_gng", [128, 4, 256], F32)
    for h in range(4):
        k.dma("sp", gng[:, h, :], I["l1_gnorm_g"].to_broadcast([128, 256]))
    oa = Ring([k.sb(s, "g_oa%d" % i, [128, 1024], F32) for i in range(2)])
    ob_ = Ring([k.sb(s, "g_ob%d" % i, [128, 1024], F32) for i in range(2)])
    go = Ring([k.sb(s, "g_go%d" % i, [128, 1024], BF16) for i in range(2)])
    o2 = Ring([k.sb(s, "g_o2%d" % i, [128, 1024], BF16) for i in range(2)])
    st = Ring([k.sb(s, "g_st%d" % i, [128, 12], F32) for i in range(2)])
    junk = k.sb(s, "g_junk", [128, 256], BF16)
    mixr = Ring([k.sb(s, "g_mix%d" % i, [128, 8, 512], BF16) for i in range(2)])
    jlast = []

    def get_mix(bi, s0, n):
        R = k.R_cur
        m_i, mb, mdeps = mixr.get()
        p_i, pT, pdeps = R["pT"].get()
        ttr = []
        for t in range(n // 128):
            r0 = s0 - TC + t * 128
            a_i, ab, adeps = oa.get()
            b_i, bb, bdeps = ob_.get()
            g_i, gb, gdeps = go.get()
            d1 = k.dma("sp", ab[:], O_d[0, r0:r0 + 128, :], deps=adeps)
            d2 = k.dma("sp", bb[:], O_d[1, r0:r0 + 128, :], deps=bdeps)
            d3 = k.dma("sp", gb[:], GO_d[s0 + t * 128:s0 + (t + 1) * 128, :], deps=gdeps)
            t0 = k.tt("pool", ab[:], ab[:], bb[:], ALU.add, deps=[d1, d2])
            ob_.release(b_i, [t0])
            s_i, sb_, sdeps = st.get()
            tq = []
            for h in range(4):
                tq.append(k.act(junk[:], ab[:, h * 256:(h + 1) * 256], AF.Square, deps=[t0] + sdeps + jlast, accum_out=sb_[:, h:h + 1]))
                jlast[:] = [tq[-1]]
            t1 = k.act(sb_[:, 4:8], sb_[:, 0:4], AF.Sqrt, deps=tq, scale=1.0 / 256, bias=k.eps[:])
            t2 = k.recip(sb_[:, 8:12], sb_[:, 4:8], deps=[t1])
            t3 = None
            for h in range(4):
                t3 = k.stt("dve", ab[:, h * 256:(h + 1) * 256], ab[:, h * 256:(h + 1) * 256], sb_[:, 8 + h:9 + h], gng[:, h, :],
                           ALU.mult, ALU.mult, deps=[t2])
            st.release(s_i, [t3])
            o_i, o2b, o2deps = o2.get()
            t4 = k.tt("pool", o2b[:], ab[:], gb[:], ALU.mult, deps=[t3, d3] + o2deps)
            oa.release(a_i, [t4]); go.release(g_i, [t4])
            last = None
            for c in range(8):
                last = k.tr(pT[:, c, t * 128:(t + 1) * 128], o2b[:, c * 128:(c + 1) * 128], k.identb[:],
                            deps=([t4] + pdeps) if c == 0 else (), sig=(c == 7))
            o2.release(o_i, [last])
            ttr.append(last)
        te = k.cp("dve", mb[:, :, 0:n], pT[:, :, 0:n], deps=ttr + mdeps)
        R["pT"].release(p_i, [te])
        return mb, [te], (lambda toks, i=m_i: mixr.release(i, toks))
    return get_mix


def kernel(**inputs):
    nc = build()
    maps = [host_inputs(inputs, b) for b in range(8)]
    res = run_bass_kernel_spmd(nc, maps, core_ids=list(range(8)))
    return np.stack([np.asarray(res.results[b]["out"], np.float32) for b in range(8)], 0)
```
